# Optimizing a Trainium2 kernel written in Bass

```python
import math
import jax, jax.numpy as jnp
from jax import lax
import numpy as np

D_MODEL = 2048
BATCH = 1
SEQ = 16384
DEPTH = 1

CHUNK = 64
Q_BLOCK = 128
D_CONV = D_MODEL // 2
D_ATTN = D_MODEL - D_CONV
N_HEADS = 8
HEAD_DIM = D_ATTN // N_HEADS // 2
V_DIM = 2 * HEAD_DIM
CONV_WIDTH = 31
N_GROUPS = 4
EXPERTS_PER_GROUP = 8
N_EXPERTS = N_GROUPS * EXPERTS_PER_GROUP
TOP_K = 2
D_EXPERT = 512
D_Q = N_HEADS * 2 * HEAD_DIM
D_V = N_HEADS * V_DIM
D_IN = 2 * D_CONV + 2 * D_Q + D_V
EPS = 1e-6

kernel_name = "hymba_conformer_diffattn_hiermoe_block"


def rms_norm(x, g):
    xf = x.astype(jnp.float32)
    y = xf * lax.rsqrt(jnp.mean(xf * xf, axis=-1, keepdims=True) + EPS)
    return (y * g.astype(jnp.float32)).astype(x.dtype)


def layer_norm(x, g, b):
    xf = x.astype(jnp.float32)
    mu = jnp.mean(xf, axis=-1, keepdims=True)
    xc = xf - mu
    y = xc * lax.rsqrt(jnp.mean(xc * xc, axis=-1, keepdims=True) + EPS)
    return (y * g.astype(jnp.float32) + b.astype(jnp.float32)).astype(x.dtype)


def lambda_init(layer_idx):
    return 0.8 - 0.6 * math.exp(-0.3 * layer_idx)


def conformer_conv(u, dw_kernel, dw_bias, ln_g, ln_b):
    a, b = jnp.split(u, 2, axis=-1)
    h = a * jax.nn.sigmoid(b)
    h = lax.conv_general_dilated(
        h, dw_kernel[:, None, :], window_strides=(1,),
        padding=[(CONV_WIDTH - 1, 0)],
        dimension_numbers=("NWC", "WIO", "NWC"),
        feature_group_count=D_CONV) + dw_bias
    h = layer_norm(h, ln_g, ln_b)
    return jax.nn.silu(h)


def diff_attention(qkv, q_norm_g, k_norm_g, lq1, lk1, lq2, lk2, subln_g, lam_init):
    B, S, _ = qkv.shape
    q, k, v = jnp.split(qkv, [D_Q, 2 * D_Q], axis=-1)
    q = q.reshape(B, S, N_HEADS, 2, HEAD_DIM)
    k = k.reshape(B, S, N_HEADS, 2, HEAD_DIM)
    v = v.reshape(B, S, N_HEADS, V_DIM)
    q = rms_norm(q, q_norm_g) * (HEAD_DIM ** -0.5)
    k = rms_norm(k, k_norm_g)
    lam = (jnp.exp(jnp.sum(lq1.astype(jnp.float32) * lk1.astype(jnp.float32)))
           - jnp.exp(jnp.sum(lq2.astype(jnp.float32) * lk2.astype(jnp.float32)))
           + lam_init)
    n_blk = S // Q_BLOCK
    qb = jnp.moveaxis(q.reshape(B, n_blk, Q_BLOCK, N_HEADS, 2, HEAD_DIM), 1, 0)
    k_chunk = jnp.arange(S) // CHUNK

    def one_block(args):
        i, q_i = args
        q_chunk = (i * Q_BLOCK + jnp.arange(Q_BLOCK)) // CHUNK
        mask = k_chunk[None, :] <= q_chunk[:, None]
        s = jnp.einsum("bqhrd,bkhrd->bhrqk", q_i, k).astype(jnp.float32)
        s = jnp.where(mask, s, -jnp.inf)
        p = jax.nn.softmax(s, axis=-1)
        a = p[:, :, 0] - lam * p[:, :, 1]
        return jnp.einsum("bhqk,bkhd->bqhd", a.astype(v.dtype), v)

    o = lax.map(one_block, (jnp.arange(n_blk), qb))
    o = jnp.moveaxis(o, 0, 1).reshape(B, S, N_HEADS, V_DIM)
    o = rms_norm(o, subln_g) * (1.0 - lam_init)
    return o.reshape(B, S, D_V)


def hier_moe(x, w_group, b_group, w_router, b_router, w_gate, w_up, w_down):
    B, S, D = x.shape
    t = x.reshape(B * S, D)
    g_logits = (t @ w_group + b_group).astype(jnp.float32)
    g_prob = jax.nn.softmax(g_logits, axis=-1)
    g_idx = jnp.argmax(g_logits, axis=-1)
    g_w = jnp.take_along_axis(g_prob, g_idx[:, None], axis=-1)
    e_logits = (jnp.einsum("nd,gde->nge", t, w_router) + b_router).astype(jnp.float32)
    e_logits = jnp.take_along_axis(e_logits, g_idx[:, None, None], axis=1)[:, 0]
    top_v, top_i = lax.top_k(e_logits, TOP_K)
    top_p = jax.nn.softmax(top_v, axis=-1)
    expert_id = g_idx[:, None] * EXPERTS_PER_GROUP + top_i
    combine = jnp.sum(jax.nn.one_hot(expert_id, N_EXPERTS, dtype=jnp.float32)
                      * (g_w * top_p)[..., None], axis=1)
    out = jnp.zeros_like(t)
    for e in range(N_EXPERTS):
        h = jax.nn.silu(t @ w_gate[e]) * (t @ w_up[e])
        out = out + combine[:, e:e + 1].astype(t.dtype) * (h @ w_down[e])
    return out.reshape(B, S, D)


def setup_inputs(seed: int = 0) -> dict:
    key = jax.random.key(seed)
    ks = jax.random.split(key, 24)
    nrm = lambda k, shape, s: jax.random.normal(k, shape, jnp.float32) * s
    L = DEPTH
    return {
        "x": nrm(ks[0], (BATCH, SEQ, D_MODEL), 1.0),
        "norm1_g": 1.0 + nrm(ks[1], (L, D_MODEL), 0.02),
        "w_in": nrm(ks[2], (L, D_MODEL, D_IN), D_MODEL ** -0.5),
        "conv_dw_kernel": nrm(ks[3], (L, CONV_WIDTH, D_CONV), CONV_WIDTH ** -0.5),
        "conv_dw_bias": nrm(ks[4], (L, D_CONV), 0.02),
        "conv_ln_g": 1.0 + nrm(ks[5], (L, D_CONV), 0.02),
        "conv_ln_b": nrm(ks[6], (L, D_CONV), 0.02),
        "q_norm_g": 1.0 + nrm(ks[7], (L, 2, HEAD_DIM), 0.02),
        "k_norm_g": 1.0 + nrm(ks[8], (L, 2, HEAD_DIM), 0.02),
        "lambda_q1": nrm(ks[9], (L, HEAD_DIM), 0.1),
        "lambda_k1": nrm(ks[10], (L, HEAD_DIM), 0.1),
        "lambda_q2": nrm(ks[11], (L, HEAD_DIM), 0.1),
        "lambda_k2": nrm(ks[12], (L, HEAD_DIM), 0.1),
        "subln_g": 1.0 + nrm(ks[13], (L, V_DIM), 0.02),
        "w_out": nrm(ks[14], (L, D_CONV + D_V, D_MODEL), (D_CONV + D_V) ** -0.5),
        "norm2_g": 1.0 + nrm(ks[15], (L, D_MODEL), 0.02),
        "w_group": nrm(ks[16], (L, D_MODEL, N_GROUPS), D_MODEL ** -0.5),
        "b_group": nrm(ks[17], (L, N_GROUPS), 0.01),
        "w_router": nrm(ks[18], (L, N_GROUPS, D_MODEL, EXPERTS_PER_GROUP), D_MODEL ** -0.5),
        "b_router": nrm(ks[19], (L, N_GROUPS, EXPERTS_PER_GROUP), 0.01),
        "w_gate": nrm(ks[20], (L, N_EXPERTS, D_MODEL, D_EXPERT), D_MODEL ** -0.5),
        "w_up": nrm(ks[21], (L, N_EXPERTS, D_MODEL, D_EXPERT), D_MODEL ** -0.5),
        "w_down": nrm(ks[22], (L, N_EXPERTS, D_EXPERT, D_MODEL), D_EXPERT ** -0.5),
    }


def reference(x, norm1_g, w_in, conv_dw_kernel, conv_dw_bias, conv_ln_g, conv_ln_b,
              q_norm_g, k_norm_g, lambda_q1, lambda_k1, lambda_q2, lambda_k2, subln_g,
              w_out, norm2_g, w_group, b_group, w_router, b_router, w_gate, w_up, w_down):
    h = x
    for l in range(DEPTH):
        u = rms_norm(h, norm1_g[l]) @ w_in[l]
        u_conv, u_attn = jnp.split(u, [2 * D_CONV], axis=-1)
        y_conv = conformer_conv(u_conv, conv_dw_kernel[l], conv_dw_bias[l],
                                conv_ln_g[l], conv_ln_b[l])
        y_attn = diff_attention(u_attn, q_norm_g[l], k_norm_g[l], lambda_q1[l], lambda_k1[l],
                                lambda_q2[l], lambda_k2[l], subln_g[l], lambda_init(l))
        h = h + jnp.concatenate([y_conv, y_attn], axis=-1) @ w_out[l]
        h = h + hier_moe(rms_norm(h, norm2_g[l]), w_group[l], b_group[l], w_router[l],
                         b_router[l], w_gate[l], w_up[l], w_down[l])
    return h
```

```python
import numpy as np
import concourse.bass as bass
import concourse.mybir as mybir
from concourse.bass_utils import run_bass_kernel_spmd
from contextlib import ExitStack

F32 = mybir.dt.float32
BF16 = mybir.dt.bfloat16
ALU = mybir.AluOpType
AF = mybir.ActivationFunctionType
AX = mybir.AxisListType

S_FULL = 16384
D = 2048
NCORE = 8
HALO = 128
EPS = 1e-6
NEXP = 32
DEXP = 512
LAM_INIT = 0.2
SEM_LIMIT = 30000


class Buf:
    def __init__(self, name):
        self.name = name
        self.last_w = None
        self.readers = []
        self.chan = None


class Op:
    __slots__ = ("eng", "fn", "deps", "flagged", "sem", "val", "kind", "chan", "idx")

    def __init__(self, eng, fn, kind):
        self.eng = eng
        self.fn = fn
        self.deps = []
        self.flagged = False
        self.sem = None
        self.val = 0
        self.kind = kind
        self.chan = None


class Chan:
    def __init__(self, sem):
        self.sem = sem
        self.count = 0


class Prog:
    ENGS = ("pe", "act", "dve", "pool", "sp")

    def __init__(self, nc, stack):
        self.nc = nc
        self.stack = stack
        self.ops = {e: [] for e in self.ENGS}
        self.nsem = 0
        self.bar_deps = {e: [] for e in self.ENGS}
        self.dma_since_bar = []
        self.free_chans = []
        self.phase_bufs = []

    def new_sem(self, name):
        self.nsem += 1
        return self.stack.enter_context(self.nc.semaphore(f"{name}_{self.nsem}"))

    def add(self, eng, fn, reads=(), writes=(), kind="c", chan_buf=None):
        op = Op(eng, fn, kind)
        deps = []
        if self.bar_deps[eng]:
            deps.extend(self.bar_deps[eng])
            self.bar_deps[eng] = []
        for b in reads:
            if b.last_w is not None:
                deps.append(b.last_w)
        for b in writes:
            if b.last_w is not None:
                deps.append(b.last_w)
            deps.extend(b.readers)
        for b in writes:
            b.last_w = op
            b.readers = []
        for b in reads:
            if b.last_w is op:
                continue
            if kind == "c":
                b.readers = [r for r in b.readers if not (r.kind == "c" and r.eng == eng)]
            b.readers.append(op)
        seen = set()
        for d in deps:
            if d is op or id(d) in seen:
                continue
            seen.add(id(d))
            if d.kind == "c" and d.eng == "pe" and eng == "pe" and kind == "c":
                continue
            op.deps.append(d)
            d.flagged = True
        if kind == "d":
            cb = chan_buf
            if cb.chan is None:
                cb.chan = self.free_chans.pop() if self.free_chans else Chan(self.new_sem("dq"))
                self.phase_bufs.append(cb)
            cb.chan.count += 1
            op.chan = cb.chan
            op.sem = cb.chan.sem
            op.val = cb.chan.count * 16
            self.dma_since_bar.append(op)
        elif kind == "cc":
            op.sem = self.new_sem("cc")
            op.val = 1
        self.ops[eng].append(op)
        return op

    def barrier(self):
        lasts = []
        for e in self.ENGS:
            for op in reversed(self.ops[e]):
                if op.kind != "d":
                    lasts.append(op)
                    op.flagged = True
                    break
        lasts.extend(self.dma_since_bar)
        self.dma_since_bar = []
        for e in self.ENGS:
            self.bar_deps[e] = self.bar_deps[e] + list(lasts)
        for b in self.phase_bufs:
            self.free_chans.append(b.chan)
            b.chan = None
        self.phase_bufs = []

    def finalize(self):
        for e in self.ENGS:
            cnt = 0
            sems = []
            for op in self.ops[e]:
                if op.kind == "c" and op.flagged:
                    si = cnt // SEM_LIMIT
                    if si >= len(sems):
                        sems.append(self.new_sem("e" + e))
                    op.sem = sems[si]
                    op.val = cnt % SEM_LIMIT + 1
                    cnt += 1

    def emit(self, eng, h):
        waited = {}
        for op in self.ops[eng]:
            need = {}
            for d in op.deps:
                if need.get(d.sem, 0) < d.val:
                    need[d.sem] = d.val
            for s, v in need.items():
                if waited.get(s, 0) < v:
                    h.wait_ge(s, v)
                    waited[s] = v
            ins = op.fn(h)
            if op.kind == "d":
                ins.then_inc(op.sem, 16)
            elif op.kind == "cc":
                ins.then_inc(op.sem)
            elif op.flagged:
                ins.then_inc(op.sem, 1)


class _Stop(Exception):
    pass


def build_program(S=S_FULL, NEXP_RUN=NEXP, stop_after=None):
    NT = S // NCORE
    NOWN = NT + HALO
    NGRP = NT // 512
    nc = bass.Bass("TRN2", target_bir_lowering=False)
    stack = ExitStack()
    P = Prog(nc, stack)

    def din(name, shape, dt=F32):
        return nc.dram_tensor(name, list(shape), dt, kind="ExternalInput")

    NTILE = S // 128
    x_keys = din("x_keys", [S, D])
    x_own = din("x_own", [NOWN, D])
    w_q = din("w_q", [D, 1024])
    w_k = din("w_k", [D, 1024])
    w_v = din("w_v", [D, 1024])
    kbias = din("kbias", [128, NTILE])
    w_conv = din("w_conv", [D, 2048])
    g1 = din("g1", [128, 16])
    cwk = din("cwk", [1024, 31])
    cbias = din("cbias", [128, 8])
    lng = din("lng", [128, 8])
    lnb = din("lnb", [128, 8])
    qkg = din("qkg", [1, 256])
    lam4 = din("lam4", [1, 256])
    sgn = din("sgn", [128, 1])
    w_out = din("w_out", [D, D])
    g2 = din("g2", [128, 16])
    w_rt = din("w_rt", [128, 16 * 36])
    b_rt = din("b_rt", [1, 36])
    w_gate = din("w_gate", [NEXP_RUN, D, DEXP])
    w_up = din("w_up", [NEXP_RUN, D, DEXP])
    w_down = din("w_down", [NEXP_RUN, DEXP, D])
    ident_in = din("ident", [128, 128])
    masks_in = din("masks", [128, 4 * 512])
    y_out = nc.dram_tensor("y", [NT, D], F32, kind="ExternalOutput")
    k_scr = nc.dram_tensor("k_scr", [8, 128, S], BF16)
    v_scr = nc.dram_tensor("v_scr", [8, 128, S], BF16)
    q_scr = nc.dram_tensor("q_scr", [8, 128, NT], BF16)
    ya_scr = nc.dram_tensor("ya_scr", [1024, NT], BF16)
    h_scr = nc.dram_tensor("h_scr", [NT, D], F32)

    ARENA_BYTES = 206 * 1024
    arena = stack.enter_context(nc.sbuf_tensor("arena", [128, ARENA_BYTES // 2], BF16))
    psb_t = [stack.enter_context(nc.psum_tensor(f"ps{i}", [128, 512], F32)) for i in range(8)]
    psb = [t[:, :] for t in psb_t]
    PS = [Buf(f"ps{i}") for i in range(8)]

    class Arena:
        def __init__(self):
            self.off = 0

        def alloc(self, nbytes, dt):
            a = self.off
            self.off += (nbytes + 63) // 64 * 64
            assert self.off <= ARENA_BYTES, f"arena overflow {self.off}"
            ap = arena[:, a // 2:(a + nbytes) // 2]
            if dt == F32:
                ap = ap.bitcast(F32)
            return ap

        def f32(self, n):
            return self.alloc(n * 4, F32)

        def bf(self, n):
            return self.alloc(n * 2, BF16)

    A = Arena()
    ident_f = A.f32(128)
    ident_b = A.bf(128)
    ones_f = A.f32(128)
    ones_b = A.bf(128)
    nhalf = A.f32(512)
    col = A.f32(64)
    B_const = Buf("const")
    CONST_END = None

    def dma(eng, out, in_, reads, writes, chan_buf):
        return P.add(eng, lambda h: h.dma_start(out=out, in_=in_), reads=reads, writes=writes,
                     kind="d", chan_buf=chan_buf)

    B_identf = Buf("identf")
    dma("sp", ident_f, ident_in.ap(), [], [B_identf], B_identf)
    P.add("dve", lambda h: h.tensor_copy(ident_b, ident_f), reads=[B_identf], writes=[B_const])
    P.add("dve", lambda h: h.memset(ones_f, 1.0), writes=[B_const])
    P.add("dve", lambda h: h.memset(ones_b, 1.0), writes=[B_const])
    P.add("dve", lambda h: h.memset(nhalf, -0.5), writes=[B_const])
    CONST_END = A.off

    final_stores = []
    try:
        def rstd_ops(v_ap, out_ap, n, bufs_in, buf_out):
            P.add("pool", lambda h: h.tensor_tensor(out_ap, v_ap, nhalf[:, 0:n], ALU.pow),
                  reads=bufs_in + [B_const], writes=[buf_out])

        qkgt = A.f32(256)
        l4 = A.f32(256)
        lamt = A.f32(8)
        sgcol = A.f32(1)
        masks = A.bf(4 * 512)
        kbt = A.f32(NTILE)
        g1col = A.f32(16)
        CONST_END = A.off

        Wq = A.bf(16 * 1024)
        Wk = A.bf(16 * 1024)
        Wv = A.bf(16 * 1024)
        xs = [A.f32(D) for _ in range(2)]
        junk = A.bf(D)
        xn = [A.bf(D) for _ in range(2)]
        xnT = [A.bf(D) for _ in range(2)]
        smalls = [A.f32(64) for _ in range(2)]
        sqk = A.f32(1024)
        tmpk = A.f32(1024)
        kb_ = A.bf(1024)
        qb_ = A.bf(1024)
        kst = A.bf(8 * 512)
        vst = A.bf(8 * 512)
        qst = A.bf(8 * 512)

        B_xs = [Buf(f"xs{i}") for i in range(2)]
        B_junk = Buf("junk")
        B_xn = [Buf(f"xn{i}") for i in range(2)]
        B_xnT = [Buf(f"xnT{i}") for i in range(2)]
        B_Wq, B_Wk, B_Wv = Buf("Wq"), Buf("Wk"), Buf("Wv")
        B_g1col = Buf("g1col")
        B_sm = [Buf(f"sm{i}") for i in range(2)]
        B_sqk = Buf("sqk")
        B_tmpk = Buf("tmpk")
        B_kb = Buf("kb")
        B_qb = Buf("qb")
        B_kst, B_vst, B_qst = Buf("kst"), Buf("vst"), Buf("qst")
        B_misc = Buf("miscAB")

        dma("sp", g1col, g1.ap(), [], [B_g1col], B_g1col)
        dma("sp", qkgt, qkg.ap()[0:1, :].broadcast_to([128, 256]), [], [B_misc], B_misc)
        B_l4 = Buf("l4")
        dma("sp", l4, lam4.ap()[0:1, :].broadcast_to([128, 256]), [], [B_l4], B_l4)
        B_sg = Buf("sg")
        dma("sp", sgcol, sgn.ap(), [], [B_sg], B_sg)
        B_kbt = Buf("kbt")
        dma("sp", kbt, kbias.ap(), [], [B_kbt], B_kbt)
        B_mk = Buf("masks")
        dma("sp", xs[1], masks_in.ap(), [], [B_xs[1]], B_xs[1])
        P.add("dve", lambda h, src=xs[1]: h.tensor_copy(masks, src), reads=[B_xs[1]], writes=[B_mk])
        P.add("dve", lambda h: h.tensor_scalar(qkgt[:, 0:128], qkgt[:, 0:128], 0.125, None, ALU.mult),
              reads=[B_misc], writes=[B_misc])
        B_lam = Buf("lam")
        P.add("dve", lambda h: h.tensor_tensor(l4[:, 0:64], l4[:, 0:64], l4[:, 64:128], ALU.mult),
              reads=[B_l4], writes=[B_l4])
        P.add("dve", lambda h: h.tensor_tensor(l4[:, 128:192], l4[:, 128:192], l4[:, 192:256], ALU.mult),
              reads=[B_l4], writes=[B_l4])
        P.add("dve", lambda h: h.tensor_reduce(lamt[:, 0:1], l4[:, 0:64], AX.X, ALU.add),
              reads=[B_l4], writes=[B_lam])
        P.add("dve", lambda h: h.tensor_reduce(lamt[:, 1:2], l4[:, 128:192], AX.X, ALU.add),
              reads=[B_l4], writes=[B_lam])
        P.add("act", lambda h: h.activation(lamt[:, 2:4], lamt[:, 0:2], AF.Exp), reads=[B_lam], writes=[B_lam])
        P.add("dve", lambda h: h.tensor_tensor(lamt[:, 4:5], lamt[:, 3:4], lamt[:, 2:3], ALU.subtract),
              reads=[B_lam], writes=[B_lam])
        P.add("dve", lambda h: h.tensor_scalar(lamt[:, 5:6], lamt[:, 4:5], -LAM_INIT, None, ALU.add),
              reads=[B_lam], writes=[B_lam])
        neg_lam = lamt[:, 5:6]
        P.add("dve", lambda h: h.tensor_scalar(sgcol, sgcol, 1.0 - LAM_INIT, None, ALU.mult),
              reads=[B_sg], writes=[B_sg])

        wcnt = [0]

        def load_w1024(src, Wt, B_Wt):
            srcv = src.ap().rearrange("(k p) n -> p k n", p=128)
            Wtv = Wt.rearrange("p (k n) -> p k n", k=16)
            for k2 in range(8):
                s = wcnt[0] % 2
                wcnt[0] += 1
                dma("sp", xs[s].rearrange("p (k n) -> p k n", k=2), srcv[:, 2 * k2:2 * k2 + 2, :], [], [B_xs[s]], B_xs[s])
                dstv = Wtv[:, 2 * k2:2 * k2 + 2, :]
                srcs = xs[s].rearrange("p (k n) -> p k n", k=2)
                if k2 % 2 == 0:
                    P.add("act", lambda h, dstv=dstv, srcs=srcs: h.copy(dstv, srcs), reads=[B_xs[s]], writes=[B_Wt])
                else:
                    P.add("dve", lambda h, dstv=dstv, srcs=srcs: h.tensor_copy(dstv, srcs), reads=[B_xs[s]], writes=[B_Wt])

        load_w1024(w_k, Wk, B_Wk)
        load_w1024(w_v, Wv, B_Wv)
        load_w1024(w_q, Wq, B_Wq)

        def norm_transpose(x_dram_rows, xs_s, Bxs, xn_s, Bxn, sm, Bsm, jk, Bjk, gcol, B_gcol,
                           ps_a, ps_b, out_views, out_bufs):
            dma("sp", xs_s, x_dram_rows, [], [Bxs], Bxs)
            P.add("act", lambda h: h.activation(jk, xs_s, AF.Square, accum_out=sm[:, 0:1]),
                  reads=[Bxs], writes=[Bjk, Bsm])
            P.add("dve", lambda h: h.tensor_scalar(sm[:, 1:2], sm[:, 0:1], 1.0 / D, EPS, ALU.mult, ALU.add),
                  reads=[Bsm], writes=[Bsm])
            rstd_ops(sm[:, 1:2], sm[:, 2:3], 1, [Bsm], Bsm)
            P.add("act", lambda h: h.activation(xn_s, xs_s, AF.Copy, scale=sm[:, 2:3]),
                  reads=[Bxs, Bsm], writes=[Bxn])
            tpa = psb[ps_a].bitcast(BF16)
            tpb = psb[ps_b].bitcast(BF16)
            for kc in range(16):
                tp = (tpa if kc < 8 else tpb)[:, (kc % 8) * 128:(kc % 8 + 1) * 128]
                P.add("pe", lambda h, tp=tp, kc=kc: h.transpose(tp, xn_s[:, kc * 128:(kc + 1) * 128], ident_b),
                      reads=[Bxn, B_const], writes=[PS[ps_a if kc < 8 else ps_b]])
            for kc in range(16):
                tp = (tpa if kc < 8 else tpb)[:, (kc % 8) * 128:(kc % 8 + 1) * 128]
                ov = out_views[kc]
                P.add("dve", lambda h, tp=tp, kc=kc, ov=ov: h.tensor_scalar(ov, tp, gcol[:, kc:kc + 1], None, ALU.mult),
                      reads=[PS[ps_a if kc < 8 else ps_b], B_gcol], writes=out_bufs)

        def a_norm(ti):
            s = ti % 2
            xv = xnT[s].rearrange("p (k t) -> p k t", k=16)
            norm_transpose(x_keys.ap()[ti * 128:(ti + 1) * 128, :], xs[s], B_xs[s], xn[s], B_xn[s],
                           smalls[s], B_sm[s], junk, B_junk, g1col, B_g1col,
                           0, 1, [xv[:, kc, :] for kc in range(16)], [B_xnT[s]])

        def proj(ti, Wt, B_Wt, b0):
            s = ti % 2
            xv = xnT[s].rearrange("p (k t) -> p k t", k=16)
            Wtv = Wt.rearrange("p (k n) -> p k n", k=16)
            for half in range(2):
                for kc in range(16):
                    P.add("pe", lambda h, kc=kc, xv=xv, half=half, Wtv=Wtv: h.matmul(
                        psb[b0 + half], xv[:, kc, :], Wtv[:, kc, half * 512:(half + 1) * 512],
                        start=(kc == 0), stop=(kc == 15)),
                        reads=[B_xnT[s], B_Wt], writes=[PS[b0 + half]])

        def qk_norm(ti, b0, goff, dst, B_dst):
            s = ti % 2
            sm = smalls[s]
            for half in range(2):
                P.add("act", lambda h, half=half: h.activation(sqk[:, half * 512:(half + 1) * 512], psb[b0 + half], AF.Square),
                      reads=[PS[b0 + half]], writes=[B_sqk])
                P.add("dve", lambda h, half=half, sm=sm: h.tensor_reduce(
                    sm[:, 4 + 8 * half:12 + 8 * half], sqk[:, half * 512:(half + 1) * 512].rearrange("p (g d) -> p g d", g=8),
                    AX.X, ALU.add), reads=[B_sqk], writes=[B_sm[s]])
            P.add("dve", lambda h, sm=sm: h.tensor_scalar(sm[:, 20:36], sm[:, 4:20], 1.0 / 64, EPS, ALU.mult, ALU.add),
                  reads=[B_sm[s]], writes=[B_sm[s]])
            rstd_ops(sm[:, 20:36], sm[:, 36:52], 16, [B_sm[s]], B_sm[s])
            for half in range(2):
                P.add("dve", lambda h, half=half, sm=sm: h.tensor_tensor(
                    tmpk[:, half * 512:(half + 1) * 512].rearrange("p (g d) -> p g d", g=8),
                    psb[b0 + half].rearrange("p (g d) -> p g d", g=8),
                    sm[:, 36 + 8 * half:44 + 8 * half].unsqueeze(2).broadcast_to([128, 8, 64]), ALU.mult),
                    reads=[PS[b0 + half], B_sm[s]], writes=[B_tmpk])
                P.add("dve", lambda h, half=half: h.tensor_tensor(
                    dst[:, half * 512:(half + 1) * 512].rearrange("p (g f) -> p g f", g=4),
                    tmpk[:, half * 512:(half + 1) * 512].rearrange("p (g f) -> p g f", g=4),
                    qkgt[:, goff:goff + 128].unsqueeze(1).broadcast_to([128, 4, 128]), ALU.mult),
                    reads=[B_tmpk, B_misc], writes=[B_dst])

        def head_transposes(src, B_src, bank, stage, B_stage, tl):
            tq = psb[bank].bitcast(BF16)
            for hh in range(8):
                P.add("pe", lambda h, hh=hh, tq=tq: h.transpose(tq[:, hh * 128:(hh + 1) * 128], src[:, hh * 128:(hh + 1) * 128], ident_b),
                      reads=[B_src, B_const], writes=[PS[bank]])
            sv = stage.rearrange("p (g t) -> p g t", g=8)[:, :, tl * 128:(tl + 1) * 128]
            P.add("act", lambda h, tq=tq, sv=sv: h.copy(sv, tq.rearrange("p (g t) -> p g t", g=8)),
                  reads=[PS[bank]], writes=[B_stage])

        if stop_after == "A0":
            raise _Stop()
        NQT = NT // 128
        a_norm(0)
        for ti in range(NTILE):
            tl = ti % 4
            proj(ti, Wk, B_Wk, 2)
            if ti + 1 < NTILE:
                a_norm(ti + 1)
            qk_norm(ti, 2, 128, kb_, B_kb)
            proj(ti, Wv, B_Wv, 4)
            vsv = vst.rearrange("p (g t d) -> p g t d", g=8, t=4)
            for half in range(2):
                P.add("act", lambda h, half=half, tl=tl, vsv=vsv: h.copy(
                    vsv[:, 4 * half:4 * half + 4, tl, :], psb[4 + half].rearrange("p (g d) -> p g d", g=4)),
                    reads=[PS[4 + half]], writes=[B_vst])
            if ti < NQT:
                proj(ti, Wq, B_Wq, 2)
                qk_norm(ti, 2, 0, qb_, B_qb)
            head_transposes(kb_, B_kb, 6, kst, B_kst, tl)
            if ti < NQT:
                head_transposes(qb_, B_qb, 7, qst, B_qst, tl)
            if tl == 3:
                t0 = (ti - 3) * 128
                dma("sp", k_scr.ap()[:, :, t0:t0 + 512].rearrange("g p t -> p g t"), kst.rearrange("p (g t) -> p g t", g=8),
                    [B_kst], [Buf(f"kscr{ti}")], B_kst)
                dma("sp", v_scr.ap()[:, :, t0:t0 + 512].rearrange("g p t -> p g t"), vst.rearrange("p (g t) -> p g t", g=8),
                    [B_vst], [Buf(f"vscr{ti}")], B_vst)
                if ti < NQT:
                    dma("sp", q_scr.ap()[:, :, t0:t0 + 512].rearrange("g p t -> p g t"), qst.rearrange("p (g t) -> p g t", g=8),
                        [B_qst], [Buf(f"qscr{ti}")], B_qst)
        P.barrier()

        if stop_after == "A":
            raise _Stop()
        A.off = CONST_END
        kTh = [A.bf(S) for _ in range(2)]
        Vh = [A.bf(S) for _ in range(2)]
        qTh = [A.bf(NT) for _ in range(2)]
        pT = [A.bf(512) for _ in range(4)]
        dacc = [A.f32(512) for _ in range(2)]
        rd = [A.f32(512) for _ in range(2)]
        osb = [A.f32(512) for _ in range(3)]
        sqo = A.f32(512)
        vrs = A.f32(512)
        rs = A.f32(512)
        yb = [A.bf(512) for _ in range(2)]
        B_kTh = [Buf("kTh0"), Buf("kTh1")]
        B_Vh = [Buf("Vh0"), Buf("Vh1")]
        B_qTh = [Buf("qTh0"), Buf("qTh1")]
        B_pT = [Buf(f"pT{i}") for i in range(4)]
        B_dacc = [Buf("dacc0"), Buf("dacc1")]
        B_rd = [Buf(f"rd{i}") for i in range(2)]
        B_os = [Buf(f"os{i}") for i in range(3)]
        B_sqo = Buf("sqo")
        B_vrs = Buf("vrs")
        B_rs = Buf("rs")
        B_yb = [Buf(f"yb{i}") for i in range(2)]

        def load_head(hd):
            s = hd % 2
            dma("sp", kTh[s], k_scr.ap()[hd], [], [B_kTh[s]], B_kTh[s])
            dma("sp", Vh[s], v_scr.ap()[hd], [], [B_Vh[s]], B_Vh[s])
            dma("sp", qTh[s], q_scr.ap()[hd], [], [B_qTh[s]], B_qTh[s])

        cnt = 0
        load_head(0)
        for hd in range(8):
            hs = hd % 2
            if hd + 1 < 8:
                load_head(hd + 1)
            kT, Vt, qT = kTh[hs], Vh[hs], qTh[hs]
            B_kT, B_V, B_qT = B_kTh[hs], B_Vh[hs], B_qTh[hs]
            for qt in range(NT // 512):
                ktiles = list(range(4 * (qt + 1))) + list(range(NQT, NTILE))
                nk = len(ktiles)
                for ki, kt in enumerate(ktiles):
                    for r in range(2):
                        sb = cnt % 3
                        pb = cnt % 4
                        cnt += 1
                        P.add("pe", lambda h, sb=sb, r=r, kt=kt, qt=qt, kT=kT, qT=qT: h.matmul(
                            psb[sb], kT[64 * r:64 * r + 64, kt * 128:(kt + 1) * 128],
                            qT[64 * r:64 * r + 64, qt * 512:(qt + 1) * 512], start=True, stop=True),
                            reads=[B_kT, B_qT], writes=[PS[sb]])
                        if kt < NQT:
                            P.add("act", lambda h, sb=sb, pb=pb: h.activation(pT[pb], psb[sb], AF.Exp),
                                  reads=[PS[sb]], writes=[B_pT[pb]])
                            if kt >= 4 * qt:
                                j = kt - 4 * qt
                                P.add("pool", lambda h, pb=pb, j=j: h.tensor_tensor(pT[pb], pT[pb], masks[:, j * 512:(j + 1) * 512], ALU.mult),
                                      reads=[B_pT[pb], B_mk], writes=[B_pT[pb]])
                        else:
                            P.add("act", lambda h, sb=sb, pb=pb, kt=kt: h.activation(pT[pb], psb[sb], AF.Exp, bias=kbt[:, kt:kt + 1]),
                                  reads=[PS[sb], B_kbt], writes=[B_pT[pb]])
                        P.add("pe", lambda h, pb=pb, r=r, kt=kt, ki=ki, nk=nk, Vt=Vt: h.matmul(
                            psb[3 + r], Vt[:, kt * 128:(kt + 1) * 128], pT[pb], start=(ki == 0), stop=(ki == nk - 1)),
                            reads=[B_V, B_pT[pb]], writes=[PS[3 + r]])
                        if ki == 0:
                            P.add("dve", lambda h, pb=pb, r=r: h.tensor_copy(dacc[r], pT[pb]),
                                  reads=[B_pT[pb]], writes=[B_dacc[r]])
                        else:
                            P.add("dve", lambda h, pb=pb, r=r: h.tensor_tensor(dacc[r], dacc[r], pT[pb], ALU.add),
                                  reads=[B_pT[pb], B_dacc[r]], writes=[B_dacc[r]])
                for r in range(2):
                    P.add("pe", lambda h, r=r: h.matmul(psb[5 + r], ones_f, dacc[r], start=True, stop=True),
                          reads=[B_const, B_dacc[r]], writes=[PS[5 + r]])
                    P.add("dve", lambda h, r=r: h.reciprocal(rd[r], psb[5 + r]), reads=[PS[5 + r]], writes=[B_rd[r]])
                    P.add("dve", lambda h, r=r: h.tensor_tensor(osb[r], psb[3 + r], rd[r], ALU.mult),
                          reads=[PS[3 + r], B_rd[r]], writes=[B_os[r]])
                P.add("dve", lambda h: h.scalar_tensor_tensor(osb[2], osb[1], neg_lam, osb[0], ALU.mult, ALU.add),
                      reads=[B_os[0], B_os[1], B_lam], writes=[B_os[2]])
                P.add("act", lambda h: h.activation(sqo, osb[2], AF.Square), reads=[B_os[2]], writes=[B_sqo])
                P.add("pe", lambda h: h.matmul(psb[7], ones_f, sqo, start=True, stop=True),
                      reads=[B_const, B_sqo], writes=[PS[7]])
                P.add("dve", lambda h: h.tensor_scalar(vrs, psb[7], 1.0 / 128, EPS, ALU.mult, ALU.add),
                      reads=[PS[7]], writes=[B_vrs])
                rstd_ops(vrs, rs, 512, [B_vrs], B_rs)
                P.add("dve", lambda h: h.tensor_tensor(osb[2], osb[2], rs, ALU.mult),
                      reads=[B_os[2], B_rs], writes=[B_os[2]])
                ys = qt % 2
                P.add("act", lambda h, ys=ys: h.activation(yb[ys], osb[2], AF.Copy, scale=sgcol),
                      reads=[B_os[2], B_sg], writes=[B_yb[ys]])
                dma("sp", ya_scr.ap()[hd * 128:(hd + 1) * 128, qt * 512:(qt + 1) * 512], yb[ys], [B_yb[ys]],
                    [Buf(f"yascr{hd}_{qt}")], B_yb[ys])
        P.barrier()

        if stop_after == "B":
            raise _Stop()
        A.off = CONST_END
        W64 = A.bf(16 * 2048)
        xs2 = [A.f32(D) for _ in range(2)]
        junk2 = A.bf(D)
        xn2 = [A.bf(D) for _ in range(2)]
        smalls2 = [A.f32(16) for _ in range(2)]
        _r1 = A.off
        xnTg = A.bf(16 * 512)
        hTt = A.f32(8 * 544)
        _r1_end = A.off
        accs = A.f32(8 * 512)
        ycT = A.bf(8 * NT)
        assert _r1_end - _r1 >= 8 * NT * 2
        yaT = arena[:, _r1 // 2:_r1 // 2 + 8 * NT]
        sig = A.f32(512)
        sqc = A.f32(512)
        mean = A.f32(512)
        msq = A.f32(512)
        var = A.f32(512)
        rstdc = A.f32(512)
        zt = A.f32(512)
        cwT = A.f32(8 * 31)
        cbc = A.f32(8)
        lngc = A.f32(8)
        lnbc = A.f32(8)
        g1c2 = A.f32(16)
        D_END = A.off

        xs[0], xs[1] = xs2
        xn[0], xn[1] = xn2
        smalls[0], smalls[1] = smalls2
        junk = junk2
        B_xs = [Buf("xs2a"), Buf("xs2b")]
        B_xn = [Buf("xn2a"), Buf("xn2b")]
        B_sm = [Buf("sm2a"), Buf("sm2b")]
        B_junk = Buf("junk2")

        B_W64 = Buf("W64")
        B_g1c2 = Buf("g1c2")
        B_small = Buf("smallD")
        B_xnTg = Buf("xnTg")
        B_hT = Buf("hT")
        B_accs = [Buf(f"accs{i}") for i in range(8)]
        B_ycT = Buf("ycT")
        B_yaT = Buf("yaT")
        B_sig = Buf("sig")
        B_sqc = Buf("sqc")
        B_stat = Buf("stat")
        B_zt = Buf("zt")

        dma("sp", g1c2, g1.ap(), [], [B_g1c2], B_g1c2)
        dma("sp", cwT.rearrange("p (c j) -> p c j", c=8), cwk.ap().rearrange("(c p) j -> p c j", p=128), [], [B_small], B_small)
        B_s2 = Buf("s2")
        dma("sp", cbc, cbias.ap(), [], [B_s2], B_s2)
        B_s3 = Buf("s3")
        dma("sp", lngc, lng.ap(), [], [B_s3], B_s3)
        B_s4 = Buf("s4")
        dma("sp", lnbc, lnb.ap(), [], [B_s4], B_s4)

        W64v = W64.rearrange("p (k n) -> p k n", k=16)

        def load_w64(src):
            srcv = src.ap().rearrange("(k p) n -> p k n", p=128)
            for kc in range(16):
                s = kc % 2
                dma("sp", xs[s], srcv[:, kc, :], [], [B_xs[s]], B_xs[s])
                eng = "act" if kc % 2 == 0 else "dve"
                if eng == "act":
                    P.add("act", lambda h, s=s, kc=kc: h.copy(W64v[:, kc, :], xs[s]), reads=[B_xs[s]], writes=[B_W64])
                else:
                    P.add("dve", lambda h, s=s, kc=kc: h.tensor_copy(W64v[:, kc, :], xs[s]), reads=[B_xs[s]], writes=[B_W64])

        load_w64(w_conv)
        hTv = hTt.rearrange("p (c t) -> p c t", c=8)
        accv = accs.rearrange("p (c t) -> p c t", c=8)
        ycv = ycT.rearrange("p (c t) -> p c t", c=8)
        yav = yaT.rearrange("p (c t) -> p c t", c=8)
        xgv = xnTg.rearrange("p (k t) -> p k t", k=16)
        cwv = cwT.rearrange("p (c j) -> p c j", c=8)
        P.add("dve", lambda h: h.memset(hTt, 0.0), writes=[B_hT])

        groups = [(0, 128, True)] + [(HALO + 512 * j, 512, False) for j in range(NGRP)]
        for gi, (r0, ntok, is_halo) in enumerate(groups):
            for t in range(ntok // 128):
                norm_transpose(x_own.ap()[r0 + t * 128:r0 + (t + 1) * 128, :], xs[t % 2], B_xs[t % 2],
                               xn[t % 2], B_xn[t % 2], smalls[t % 2], B_sm[t % 2], junk, B_junk, g1c2, B_g1c2, 0, 1,
                               [xgv[:, kc, t * 128:(t + 1) * 128] for kc in range(16)], [B_xnTg])
            if gi >= 1:
                pn = groups[gi - 1][1]
                for c in range(8):
                    P.add("dve", lambda h, c=c, pn=pn: h.tensor_copy(hTv[:, c, 2:32], hTv[:, c, 32 + pn - 30:32 + pn]),
                          reads=[B_hT], writes=[B_hT])
            for c in range(8):
                for half in range(2):
                    pb_ = 2 + half
                    cofs = half * 1024 + c * 128
                    for kc in range(16):
                        P.add("pe", lambda h, pb_=pb_, kc=kc, cofs=cofs, ntok=ntok: h.matmul(
                            psb[pb_][:, 0:ntok], W64v[:, kc, cofs:cofs + 128], xgv[:, kc, 0:ntok],
                            start=(kc == 0), stop=(kc == 15)),
                            reads=[B_W64, B_xnTg], writes=[PS[pb_]])
                P.add("act", lambda h, ntok=ntok: h.activation(sig[:, 0:ntok], psb[3][:, 0:ntok], AF.Sigmoid),
                      reads=[PS[3]], writes=[B_sig])
                P.add("dve", lambda h, c=c, ntok=ntok: h.tensor_tensor(hTv[:, c, 32:32 + ntok], psb[2][:, 0:ntok], sig[:, 0:ntok], ALU.mult),
                      reads=[PS[2], B_sig], writes=[B_hT])
            if is_halo:
                continue
            j = gi - 1
            for c in range(8):
                eng = "dve"
                P.add(eng, lambda h, c=c: h.tensor_scalar(accv[:, c, :], hTv[:, c, 2:2 + 512], cwv[:, c, 0:1], cbc[:, c:c + 1], ALU.mult, ALU.add),
                      reads=[B_hT, B_small, B_s2], writes=[B_accs[c]])
                for k in range(1, 31):
                    P.add(eng, lambda h, c=c, k=k: h.scalar_tensor_tensor(accv[:, c, :], hTv[:, c, 2 + k:2 + k + 512], cwv[:, c, k:k + 1], accv[:, c, :], ALU.mult, ALU.add),
                          reads=[B_hT, B_small, B_accs[c]], writes=[B_accs[c]])
            for c in range(8):
                P.add("act", lambda h, c=c: h.activation(sqc, accv[:, c, :], AF.Square), reads=[B_accs[c]], writes=[B_sqc])
                P.add("pe", lambda h, c=c: h.matmul(psb[4], ones_f, accv[:, c, :], start=(c == 0), stop=(c == 7)),
                      reads=[B_const, B_accs[c]], writes=[PS[4]])
                P.add("pe", lambda h, c=c: h.matmul(psb[5], ones_f, sqc, start=(c == 0), stop=(c == 7)),
                      reads=[B_const, B_sqc], writes=[PS[5]])
            P.add("dve", lambda h: h.tensor_scalar(mean, psb[4], 1.0 / 1024, None, ALU.mult), reads=[PS[4]], writes=[B_stat])
            P.add("dve", lambda h: h.tensor_tensor(msq, mean, mean, ALU.mult), reads=[B_stat], writes=[B_stat])
            P.add("dve", lambda h: h.scalar_tensor_tensor(var, psb[5], 1.0 / 1024, msq, ALU.mult, ALU.subtract),
                  reads=[PS[5], B_stat], writes=[B_stat])
            P.add("dve", lambda h: h.tensor_scalar(var, var, EPS, None, ALU.add), reads=[B_stat], writes=[B_stat])
            rstd_ops(var, rstdc, 512, [B_stat], B_stat)
            for c in range(8):
                P.add("dve", lambda h, c=c: h.tensor_tensor(zt, accv[:, c, :], mean, ALU.subtract),
                      reads=[B_accs[c], B_stat], writes=[B_zt])
                P.add("dve", lambda h: h.tensor_tensor(zt, zt, rstdc, ALU.mult), reads=[B_zt, B_stat], writes=[B_zt])
                P.add("act", lambda h, c=c, j=j: h.activation(ycv[:, c, j * 512:(j + 1) * 512], zt, AF.Silu,
                                                            bias=lnbc[:, c:c + 1], scale=lngc[:, c:c + 1]),
                      reads=[B_zt, B_s3, B_s4], writes=[B_ycT])

        if stop_after == "D1":
            raise _Stop()
        P.barrier()
        for hh in range(8):
            dma("sp", yav[:, hh, :], ya_scr.ap()[hh * 128:(hh + 1) * 128, :], [], [B_yaT], B_yaT)

        load_w64(w_out)
        B_hs = [Buf("hs0"), Buf("hs1")]
        for tt in range(NT // 128):
            s = tt % 2
            dma("sp", xs[s], x_own.ap()[HALO + tt * 128:HALO + (tt + 1) * 128, :], [], [B_xs[s]], B_xs[s])
            for cg in range(4):
                pbk = 4 + cg
                for kc in range(16):
                    src = ycv[:, kc, tt * 128:(tt + 1) * 128] if kc < 8 else yav[:, kc - 8, tt * 128:(tt + 1) * 128]
                    P.add("pe", lambda h, pbk=pbk, kc=kc, src=src, cg=cg: h.matmul(
                        psb[pbk], src, W64v[:, kc, cg * 512:(cg + 1) * 512], start=(kc == 0), stop=(kc == 15)),
                        reads=[B_ycT, B_yaT, B_W64], writes=[PS[pbk]])
                P.add("dve", lambda h, pbk=pbk, s=s, cg=cg: h.tensor_tensor(xs[s][:, cg * 512:(cg + 1) * 512], xs[s][:, cg * 512:(cg + 1) * 512], psb[pbk], ALU.add),
                      reads=[PS[pbk], B_xs[s]], writes=[B_xs[s]])
            B_hrow = Buf(f"hrow{tt}")
            dma("sp", h_scr.ap()[tt * 128:(tt + 1) * 128, :], xs[s], [B_xs[s]], [B_hrow], B_xs[s])
        P.barrier()

        if stop_after == "D":
            raise _Stop()
        A.off = CONST_END
        NPASS = NT // 512
        TPP = 4
        acc = [A.f32(D) for _ in range(TPP)]
        tT = A.bf(16 * 512)
        Wsl = [A.bf(16 * 512) for _ in range(4)]
        st = [A.f32(D) for _ in range(3)]
        hTm = A.bf(4 * 512)
        junk3 = A.bf(D)
        tn32 = A.f32(D)
        hib = A.bf(D)
        lob = A.bf(D)
        thT = A.bf(D)
        tlT = A.bf(D)
        Wg32 = A.f32(16 * 36)
        Whb = A.bf(16 * 36)
        Wlb = A.bf(16 * 36)
        sgl = [A.f32(512) for _ in range(2)]
        cwt = A.f32(TPP * 32)
        Wr32 = A.f32(16 * 36)
        brt = A.f32(36)
        g2col = A.f32(16)
        lg = A.f32(36)
        rt = A.f32(64)
        sm3 = A.f32(8)

        B_acc = [Buf(f"acc{i}") for i in range(TPP)]
        B_tT = Buf("tT")
        B_W = [Buf(f"Wsl{i}") for i in range(4)]
        B_st = [Buf(f"st{i}") for i in range(3)]
        B_hTm = Buf("hTm")
        B_junk3 = Buf("junk3")
        B_tn32 = Buf("tn32")
        B_hib, B_lob, B_thT, B_tlT = Buf("hib"), Buf("lob"), Buf("thT"), Buf("tlT")
        B_sgl = [Buf("sgl0"), Buf("sgl1")]
        B_cwt = Buf("cwt")
        B_Wr = Buf("Wr32")
        B_brt = Buf("brt")
        B_g2 = Buf("g2col")
        B_lg = Buf("lg")
        B_rt = Buf("rt")
        B_sm3 = Buf("sm3")

        dma("sp", Wr32, w_rt.ap(), [], [B_Wr], B_Wr)
        dma("sp", brt, b_rt.ap()[0:1, :].broadcast_to([128, 36]), [], [B_brt], B_brt)
        dma("sp", g2col, g2.ap(), [], [B_g2], B_g2)
        if stop_after == "E0a":
            raise _Stop()
        Wrv = Wr32.rearrange("p (k n) -> p k n", k=16)
        tTv = tT.rearrange("p (k t) -> p k t", k=16)
        P.add("dve", lambda h: h.tensor_tensor(Wg32.rearrange("p (k n) -> p k n", k=16), Wrv,
                                               g2col.unsqueeze(2).broadcast_to([128, 16, 36]), ALU.mult),
              reads=[B_Wr, B_g2], writes=[B_Wr])
        P.add("act", lambda h: h.copy(Whb, Wg32), reads=[B_Wr], writes=[B_Wr])
        P.add("dve", lambda h: h.tensor_tensor(Wlb, Wg32, Whb, ALU.subtract), reads=[B_Wr], writes=[B_Wr])
        Whv = Whb.rearrange("p (k n) -> p k n", k=16)
        Wlv = Wlb.rearrange("p (k n) -> p k n", k=16)
        hmv = hTm.rearrange("p (f t) -> p f t", f=4)
        cwtv = cwt.rearrange("p (t e) -> p t e", t=TPP)
        wq = [0]
        sq_ = [0]
        final_stores = []

        def load_expert_matrix(src_view, npieces, piece_view_fn):
            slot = wq[0] % 4
            wq[0] += 1
            for pc in range(npieces):
                s = sq_[0] % 3
                sq_[0] += 1
                dma("sp", piece_view_fn(st[s]), src_view(pc), [], [B_st[s]], B_st[s])
                dst = Wsl[slot][:, pc * 2048:(pc + 1) * 2048]
                if pc % 2 == 0:
                    P.add("act", lambda h, s=s, dst=dst: h.copy(dst, st[s]), reads=[B_st[s]], writes=[B_W[slot]])
                else:
                    P.add("pool", lambda h, s=s, dst=dst: h.tensor_copy(dst, st[s]), reads=[B_st[s]], writes=[B_W[slot]])
            return slot

        for ps_ in range(NPASS):
            for tt in range(TPP):
                row0 = (ps_ * TPP + tt) * 128
                dma("sp", acc[tt], h_scr.ap()[row0:row0 + 128, :], [], [B_acc[tt]], B_acc[tt])
                P.add("act", lambda h, tt=tt: h.activation(junk3, acc[tt], AF.Square, accum_out=sm3[:, 0:1]),
                      reads=[B_acc[tt]], writes=[B_junk3, B_sm3])
                P.add("dve", lambda h: h.tensor_scalar(sm3[:, 1:2], sm3[:, 0:1], 1.0 / D, EPS, ALU.mult, ALU.add),
                      reads=[B_sm3], writes=[B_sm3])
                rstd_ops(sm3[:, 1:2], sm3[:, 2:3], 1, [B_sm3], B_sm3)
                P.add("act", lambda h, tt=tt: h.activation(tn32, acc[tt], AF.Copy, scale=sm3[:, 2:3]),
                      reads=[B_acc[tt], B_sm3], writes=[B_tn32])
                if stop_after == "E0b":
                    raise _Stop()
                P.add("act", lambda h: h.copy(hib, tn32), reads=[B_tn32], writes=[B_hib])
                P.add("dve", lambda h: h.tensor_tensor(lob, tn32, hib, ALU.subtract), reads=[B_tn32, B_hib], writes=[B_lob])
                for (srcb, B_srcb, bk) in ((hib, B_hib, 0), (lob, B_lob, 2)):
                    for kc in range(16):
                        tpv = psb[bk + kc // 8].bitcast(BF16)[:, (kc % 8) * 128:(kc % 8 + 1) * 128]
                        P.add("pe", lambda h, kc=kc, tpv=tpv, srcb=srcb: h.transpose(tpv, srcb[:, kc * 128:(kc + 1) * 128], ident_b),
                              reads=[B_srcb, B_const], writes=[PS[bk + kc // 8]])
                for hf in range(2):
                    P.add("dve", lambda h, hf=hf: h.tensor_copy(thT[:, hf * 1024:(hf + 1) * 1024], psb[0 + hf].bitcast(BF16)),
                          reads=[PS[0 + hf]], writes=[B_thT])
                    P.add("act", lambda h, hf=hf: h.copy(tlT[:, hf * 1024:(hf + 1) * 1024], psb[2 + hf].bitcast(BF16)),
                          reads=[PS[2 + hf]], writes=[B_tlT])
                P.add("pool", lambda h, tt=tt: h.tensor_tensor(tTv[:, :, tt * 128:(tt + 1) * 128], thT.rearrange("p (k t) -> p k t", k=16),
                                                             g2col.unsqueeze(2).broadcast_to([128, 16, 128]), ALU.mult),
                      reads=[B_thT, B_g2], writes=[B_tT])
                if stop_after == "E1a":
                    raise _Stop()
                thv = thT.rearrange("p (k t) -> p k t", k=16)
                tlv = tlT.rearrange("p (k t) -> p k t", k=16)
                combos = [(thv, B_thT, Whv), (thv, B_thT, Wlv), (tlv, B_tlT, Whv), (tlv, B_tlT, Wlv)]
                for ci, (tv_, B_tv, wv_) in enumerate(combos):
                    for kc in range(16):
                        P.add("pe", lambda h, kc=kc, tv_=tv_, wv_=wv_, ci=ci: h.matmul(
                            psb[4][:, 0:36], tv_[:, kc, :], wv_[:, kc, :], start=(ci == 0 and kc == 0), stop=(ci == 3 and kc == 15)),
                            reads=[B_tv, B_Wr], writes=[PS[4]])
                P.add("dve", lambda h: h.tensor_tensor(lg, psb[4][:, 0:36], brt, ALU.add), reads=[PS[4], B_brt], writes=[B_lg])
                if stop_after == "E1b":
                    raise _Stop()
                R = rt
                def dv(fn, reads, writes):
                    P.add("dve", fn, reads=reads, writes=writes)
                dv(lambda h: h.tensor_reduce(R[:, 0:1], lg[:, 0:4], AX.X, ALU.max), [B_lg], [B_rt])
                dv(lambda h: h.tensor_scalar(R[:, 1:2], R[:, 0:1], -1.0, None, ALU.mult), [B_rt], [B_rt])
                P.add("act", lambda h: h.activation(R[:, 56:60], lg[:, 0:4], AF.Exp, bias=R[:, 1:2], accum_out=R[:, 2:3]),
                      reads=[B_lg, B_rt], writes=[B_rt])
                dv(lambda h: h.reciprocal(R[:, 3:4], R[:, 2:3]), [B_rt], [B_rt])
                dv(lambda h: h.tensor_scalar(R[:, 4:8], lg[:, 0:4], R[:, 0:1], None, ALU.is_equal), [B_lg, B_rt], [B_rt])
                dv(lambda h: h.tensor_scalar(R[:, 8:16], lg[:, 4:12], R[:, 4:5], None, ALU.mult), [B_lg, B_rt], [B_rt])
                for g in range(1, 4):
                    dv(lambda h, g=g: h.scalar_tensor_tensor(R[:, 8:16], lg[:, 4 + 8 * g:12 + 8 * g], R[:, 4 + g:5 + g], R[:, 8:16], ALU.mult, ALU.add),
                       [B_lg, B_rt], [B_rt])
                dv(lambda h: h.tensor_reduce(R[:, 16:17], R[:, 8:16], AX.X, ALU.max), [B_rt], [B_rt])
                dv(lambda h: h.tensor_scalar(R[:, 17:25], R[:, 8:16], R[:, 16:17], None, ALU.is_equal), [B_rt], [B_rt])
                dv(lambda h: h.scalar_tensor_tensor(R[:, 25:33], R[:, 17:25], -1.0e30, R[:, 8:16], ALU.mult, ALU.add), [B_rt], [B_rt])
                dv(lambda h: h.tensor_reduce(R[:, 33:34], R[:, 25:33], AX.X, ALU.max), [B_rt], [B_rt])
                dv(lambda h: h.tensor_scalar(R[:, 34:42], R[:, 25:33], R[:, 33:34], None, ALU.is_equal), [B_rt], [B_rt])
                dv(lambda h: h.tensor_tensor(R[:, 42:43], R[:, 33:34], R[:, 16:17], ALU.subtract), [B_rt], [B_rt])
                P.add("act", lambda h: h.activation(R[:, 43:44], R[:, 42:43], AF.Exp), reads=[B_rt], writes=[B_rt])
                dv(lambda h: h.tensor_scalar(R[:, 44:45], R[:, 43:44], 1.0, None, ALU.add), [B_rt], [B_rt])
                dv(lambda h: h.reciprocal(R[:, 45:46], R[:, 44:45]), [B_rt], [B_rt])
                dv(lambda h: h.tensor_tensor(R[:, 46:47], R[:, 43:44], R[:, 45:46], ALU.mult), [B_rt], [B_rt])
                dv(lambda h: h.tensor_scalar(R[:, 48:56], R[:, 17:25], R[:, 45:46], None, ALU.mult), [B_rt], [B_rt])
                dv(lambda h: h.scalar_tensor_tensor(R[:, 48:56], R[:, 34:42], R[:, 46:47], R[:, 48:56], ALU.mult, ALU.add), [B_rt], [B_rt])
                dv(lambda h: h.tensor_scalar(R[:, 48:56], R[:, 48:56], R[:, 3:4], None, ALU.mult), [B_rt], [B_rt])
                for g in range(4):
                    dv(lambda h, g=g, tt=tt: h.tensor_scalar(cwtv[:, tt, 8 * g:8 * g + 8], R[:, 48:56], R[:, 4 + g:5 + g], None, ALU.mult),
                       [B_rt], [B_cwt])
            if stop_after == "E1":
                raise _Stop()
            for e in range(NEXP_RUN):
                gsl = load_expert_matrix(lambda pc, e=e: w_gate.ap()[e].rearrange("(k p) f -> p k f", p=128)[:, 4 * pc:4 * pc + 4, :],
                                         4, lambda stt: stt.rearrange("p (k f) -> p k f", k=4))
                usl = load_expert_matrix(lambda pc, e=e: w_up.ap()[e].rearrange("(k p) f -> p k f", p=128)[:, 4 * pc:4 * pc + 4, :],
                                         4, lambda stt: stt.rearrange("p (k f) -> p k f", k=4))
                dsl = load_expert_matrix(lambda pc, e=e: w_down.ap()[e][pc * 128:(pc + 1) * 128, :],
                                         4, lambda stt: stt)
                Wg = Wsl[gsl].rearrange("p (k f) -> p k f", k=16)
                Wu = Wsl[usl].rearrange("p (k f) -> p k f", k=16)
                Wd = Wsl[dsl].rearrange("p (f n) -> p f n", f=4)
                for fc in range(4):
                    gb = 0 + (fc % 2)
                    ub = 2 + (fc % 2)
                    for kc in range(16):
                        P.add("pe", lambda h, gb=gb, kc=kc, fc=fc, Wg=Wg: h.matmul(psb[gb], Wg[:, kc, fc * 128:(fc + 1) * 128], tTv[:, kc, :], start=(kc == 0), stop=(kc == 15)),
                              reads=[B_W[gsl], B_tT], writes=[PS[gb]])
                    for kc in range(16):
                        P.add("pe", lambda h, ub=ub, kc=kc, fc=fc, Wu=Wu: h.matmul(psb[ub], Wu[:, kc, fc * 128:(fc + 1) * 128], tTv[:, kc, :], start=(kc == 0), stop=(kc == 15)),
                              reads=[B_W[usl], B_tT], writes=[PS[ub]])
                    sl = fc % 2
                    P.add("act", lambda h, gb=gb, sl=sl: h.activation(sgl[sl], psb[gb], AF.Silu), reads=[PS[gb]], writes=[B_sgl[sl]])
                    P.add("dve", lambda h, ub=ub, sl=sl, fc=fc: h.tensor_tensor(hmv[:, fc, :], sgl[sl], psb[ub], ALU.mult),
                          reads=[B_sgl[sl], PS[ub]], writes=[B_hTm])
                oc = 0
                for tt in range(TPP):
                    for cg in range(4):
                        ob = 4 + (oc % 4)
                        oc += 1
                        for fc in range(4):
                            P.add("pe", lambda h, ob=ob, fc=fc, tt=tt, cg=cg, Wd=Wd: h.matmul(psb[ob], hmv[:, fc, tt * 128:(tt + 1) * 128], Wd[:, fc, cg * 512:(cg + 1) * 512], start=(fc == 0), stop=(fc == 3)),
                                  reads=[B_hTm, B_W[dsl]], writes=[PS[ob]])
                        P.add("dve", lambda h, ob=ob, tt=tt, cg=cg, e=e: h.scalar_tensor_tensor(
                            acc[tt][:, cg * 512:(cg + 1) * 512], psb[ob], cwtv[:, tt, e:e + 1], acc[tt][:, cg * 512:(cg + 1) * 512], ALU.mult, ALU.add),
                            reads=[PS[ob], B_cwt, B_acc[tt]], writes=[B_acc[tt]])
            for tt in range(TPP):
                row0 = (ps_ * TPP + tt) * 128
                B_yrow = Buf(f"yrow{row0}")
                final_stores.append(dma("sp", y_out.ap()[row0:row0 + 128, :], acc[tt], [B_acc[tt]], [B_yrow], B_acc[tt]))

    except _Stop:
        pass

    fin = P.add("sp", lambda h: h.nop(), reads=[], writes=[])
    for so in final_stores:
        fin.deps.append(so)

    P.finalize()
    with nc.Block() as block:
        @block.tensor
        def _(e):
            P.emit("pe", e)

        @block.scalar
        def _(e):
            P.emit("act", e)

        @block.vector
        def _(e):
            P.emit("dve", e)

        @block.gpsimd
        def _(e):
            P.emit("pool", e)

        @block.sync
        def _(e):
            P.emit("sp", e)
    stack.close()
    return nc


def _masks():
    k = np.arange(128)[:, None]
    q = np.arange(512)[None, :]
    m = np.zeros((128, 4 * 512), np.float32)
    for j in range(4):
        m[:, j * 512:(j + 1) * 512] = ((2 * j + k // 64) <= (q // 64)).astype(np.float32)
    return m


def make_in_maps(x, norm1_g, w_in, conv_dw_kernel, conv_dw_bias, conv_ln_g, conv_ln_b,
                 q_norm_g, k_norm_g, lambda_q1, lambda_k1, lambda_q2, lambda_k2, subln_g,
                 w_out, norm2_g, w_group, b_group, w_router, b_router, w_gate, w_up, w_down, nexp_run=NEXP):
    f = lambda a: np.ascontiguousarray(np.asarray(a, dtype=np.float32))
    S = int(np.asarray(x).shape[1])
    NT = S // NCORE
    NOWN = NT + HALO
    NTILE = S // 128
    x2 = f(x).reshape(S, D)
    w_in0 = f(w_in)[0]
    xpad = np.concatenate([np.zeros((HALO, D), np.float32), x2], axis=0)
    w_rt = np.concatenate([f(w_group)[0]] + [f(w_router)[0, g] for g in range(4)], axis=1)
    b_rt = np.concatenate([f(b_group)[0]] + [f(b_router)[0, g] for g in range(4)], axis=0)[None, :]
    common = dict(
        w_conv=f(w_in0[:, 0:2048]), w_q=f(w_in0[:, 2048:3072]), w_k=f(w_in0[:, 3072:4096]),
        w_v=f(w_in0[:, 4096:5120]), g1=f(f(norm1_g).reshape(16, 128).T),
        cwk=f(f(conv_dw_kernel)[0].T), cbias=f(f(conv_dw_bias).reshape(8, 128).T),
        lng=f(f(conv_ln_g).reshape(8, 128).T), lnb=f(f(conv_ln_b).reshape(8, 128).T),
        qkg=np.concatenate([f(q_norm_g).reshape(1, 128), f(k_norm_g).reshape(1, 128)], axis=1),
        lam4=np.concatenate([f(lambda_q1).reshape(1, 64), f(lambda_k1).reshape(1, 64),
                             f(lambda_q2).reshape(1, 64), f(lambda_k2).reshape(1, 64)], axis=1),
        sgn=f(f(subln_g).reshape(1, 128).T), w_out=f(w_out)[0], g2=f(f(norm2_g).reshape(16, 128).T),
        w_rt=f(w_rt.reshape(16, 128, 36).transpose(1, 0, 2).reshape(128, 16 * 36)), b_rt=f(b_rt), w_gate=f(f(w_gate)[0][:nexp_run]), w_up=f(f(w_up)[0][:nexp_run]),
        w_down=f(f(w_down)[0][:nexp_run]),
        ident=np.eye(128, dtype=np.float32), masks=_masks(),
    )
    maps = []
    for c in range(NCORE):
        m = dict(common)
        m["x_own"] = f(xpad[c * NT:c * NT + NOWN])
        order = [c] + [b for b in range(NCORE) if b != c]
        m["x_keys"] = f(np.concatenate([x2[b * NT:(b + 1) * NT] for b in order], axis=0))
        kb = np.zeros((128, NTILE), np.float32)
        for pos, b in enumerate(order):
            if b > c:
                kb[:, pos * (NT // 128):(pos + 1) * (NT // 128)] = -30000.0
        m["kbias"] = kb
        maps.append(m)
    return maps


_NC_CACHE = {}


def kernel(**inputs):
    if "nc" not in _NC_CACHE:
        _NC_CACHE["nc"] = build_program()
    nc = _NC_CACHE["nc"]
    maps = make_in_maps(**inputs)
    res = run_bass_kernel_spmd(nc, maps, core_ids=list(range(NCORE)))
    out = np.concatenate([np.asarray(r["y"], dtype=np.float32) for r in res.results], axis=0)
    return out.reshape(1, S_FULL, D)
```

```python
import numpy as np
import concourse.bass as bass
import concourse.mybir as mybir
from concourse.bass_utils import run_bass_kernel_spmd
from contextlib import ExitStack

F32 = mybir.dt.float32
BF16 = mybir.dt.bfloat16
ALU = mybir.AluOpType
AF = mybir.ActivationFunctionType
AX = mybir.AxisListType

S_FULL = 16384
D = 2048
NCORE = 8
HALO = 128
EPS = 1e-6
NEXP = 32
DEXP = 512
LAM_INIT = 0.2
SEM_LIMIT = 30000


class Buf:
    def __init__(self, name):
        self.name = name
        self.last_w = None
        self.readers = []
        self.chan = None


class Op:
    __slots__ = ("eng", "fn", "deps", "flagged", "sem", "val", "kind", "chan", "idx")

    def __init__(self, eng, fn, kind):
        self.eng = eng
        self.fn = fn
        self.deps = []
        self.flagged = False
        self.sem = None
        self.val = 0
        self.kind = kind
        self.chan = None


class Chan:
    def __init__(self, sem):
        self.sem = sem
        self.count = 0


class Prog:
    ENGS = ("pe", "act", "dve", "pool", "sp")

    def __init__(self, nc, stack):
        self.nc = nc
        self.stack = stack
        self.ops = {e: [] for e in self.ENGS}
        self.nsem = 0
        self.bar_deps = {e: [] for e in self.ENGS}
        self.dma_since_bar = []
        self.free_chans = []
        self.phase_bufs = []

    def new_sem(self, name):
        self.nsem += 1
        return self.stack.enter_context(self.nc.semaphore(f"{name}_{self.nsem}"))

    def add(self, eng, fn, reads=(), writes=(), kind="c", chan_buf=None):
        op = Op(eng, fn, kind)
        deps = []
        if self.bar_deps[eng]:
            deps.extend(self.bar_deps[eng])
            self.bar_deps[eng] = []
        for b in reads:
            if b.last_w is not None:
                deps.append(b.last_w)
        for b in writes:
            if b.last_w is not None:
                deps.append(b.last_w)
            deps.extend(b.readers)
        for b in writes:
            b.last_w = op
            b.readers = []
        for b in reads:
            if b.last_w is op:
                continue
            if kind == "c":
                b.readers = [r for r in b.readers if not (r.kind == "c" and r.eng == eng)]
            b.readers.append(op)
        seen = set()
        for d in deps:
            if d is op or id(d) in seen:
                continue
            seen.add(id(d))
            if d.kind == "c" and d.eng == "pe" and eng == "pe" and kind == "c":
                continue
            op.deps.append(d)
            d.flagged = True
        if kind == "d":
            cb = chan_buf
            if cb.chan is None:
                cb.chan = self.free_chans.pop() if self.free_chans else Chan(self.new_sem("dq"))
                self.phase_bufs.append(cb)
            cb.chan.count += 1
            op.chan = cb.chan
            op.sem = cb.chan.sem
            op.val = cb.chan.count * 16
            self.dma_since_bar.append(op)
        elif kind == "cc":
            op.sem = self.new_sem("cc")
            op.val = 1
        self.ops[eng].append(op)
        return op

    def barrier(self):
        lasts = []
        for e in self.ENGS:
            for op in reversed(self.ops[e]):
                if op.kind != "d":
                    lasts.append(op)
                    op.flagged = True
                    break
        lasts.extend(self.dma_since_bar)
        self.dma_since_bar = []
        for e in self.ENGS:
            self.bar_deps[e] = self.bar_deps[e] + list(lasts)
        for b in self.phase_bufs:
            self.free_chans.append(b.chan)
            b.chan = None
        self.phase_bufs = []

    def finalize(self):
        for e in self.ENGS:
            cnt = 0
            sems = []
            for op in self.ops[e]:
                if op.kind == "c" and op.flagged:
                    si = cnt // SEM_LIMIT
                    if si >= len(sems):
                        sems.append(self.new_sem("e" + e))
                    op.sem = sems[si]
                    op.val = cnt % SEM_LIMIT + 1
                    cnt += 1

    def emit(self, eng, h):
        waited = {}
        for op in self.ops[eng]:
            need = {}
            for d in op.deps:
                if need.get(d.sem, 0) < d.val:
                    need[d.sem] = d.val
            for s, v in need.items():
                if waited.get(s, 0) < v:
                    h.wait_ge(s, v)
                    waited[s] = v
            ins = op.fn(h)
            if op.kind == "d":
                ins.then_inc(op.sem, 16)
            elif op.kind == "cc":
                ins.then_inc(op.sem)
            elif op.flagged:
                ins.then_inc(op.sem, 1)


class _Stop(Exception):
    pass


def build_program(S=S_FULL, NEXP_RUN=NEXP, stop_after=None):
    NT = S // NCORE
    NOWN = NT + HALO
    NGRP = NT // 512
    nc = bass.Bass("TRN2", target_bir_lowering=False)
    stack = ExitStack()
    P = Prog(nc, stack)

    def din(name, shape, dt=F32):
        return nc.dram_tensor(name, list(shape), dt, kind="ExternalInput")

    NTILE = S // 128
    x_keys = din("x_keys", [S, D])
    x_own = din("x_own", [NOWN, D])
    w_q = din("w_q", [D, 1024])
    w_k = din("w_k", [D, 1024])
    w_v = din("w_v", [D, 1024])
    kbias = din("kbias", [128, NTILE])
    w_conv = din("w_conv", [D, 2048])
    g1 = din("g1", [128, 16])
    cwk = din("cwk", [1024, 31])
    cbias = din("cbias", [128, 8])
    lng = din("lng", [128, 8])
    lnb = din("lnb", [128, 8])
    qkg = din("qkg", [1, 256])
    lam4 = din("lam4", [1, 256])
    sgn = din("sgn", [128, 1])
    w_out = din("w_out", [D, D])
    g2 = din("g2", [128, 16])
    w_rt = din("w_rt", [128, 16 * 36])
    b_rt = din("b_rt", [1, 36])
    w_gate = din("w_gate", [NEXP_RUN, D, DEXP])
    w_up = din("w_up", [NEXP_RUN, D, DEXP])
    w_down = din("w_down", [NEXP_RUN, DEXP, D])
    ident_in = din("ident", [128, 128])
    masks_in = din("masks", [128, 4 * 512])
    y_out = nc.dram_tensor("y", [NT, D], F32, kind="ExternalOutput")
    k_scr = nc.dram_tensor("k_scr", [8, 128, S], BF16)
    v_scr = nc.dram_tensor("v_scr", [8, 128, S], BF16)
    q_scr = nc.dram_tensor("q_scr", [8, 128, NT], BF16)
    ya_scr = nc.dram_tensor("ya_scr", [1024, NT], BF16)
    h_scr = nc.dram_tensor("h_scr", [NT, D], F32)

    ARENA_BYTES = 206 * 1024
    arena = stack.enter_context(nc.sbuf_tensor("arena", [128, ARENA_BYTES // 2], BF16))
    psb_t = [stack.enter_context(nc.psum_tensor(f"ps{i}", [128, 512], F32)) for i in range(8)]
    psb = [t[:, :] for t in psb_t]
    PS = [Buf(f"ps{i}") for i in range(8)]

    class Arena:
        def __init__(self):
            self.off = 0

        def alloc(self, nbytes, dt):
            a = self.off
            self.off += (nbytes + 63) // 64 * 64
            assert self.off <= ARENA_BYTES, f"arena overflow {self.off}"
            ap = arena[:, a // 2:(a + nbytes) // 2]
            if dt == F32:
                ap = ap.bitcast(F32)
            return ap

        def f32(self, n):
            return self.alloc(n * 4, F32)

        def bf(self, n):
            return self.alloc(n * 2, BF16)

    A = Arena()
    ident_f = A.f32(128)
    ident_b = A.bf(128)
    ones_f = A.f32(128)
    ones_b = A.bf(128)
    nhalf = A.f32(512)
    col = A.f32(64)
    B_const = Buf("const")
    CONST_END = None

    def dma(eng, out, in_, reads, writes, chan_buf):
        return P.add(eng, lambda h: h.dma_start(out=out, in_=in_), reads=reads, writes=writes,
                     kind="d", chan_buf=chan_buf)

    B_identf = Buf("identf")
    dma("sp", ident_f, ident_in.ap(), [], [B_identf], B_identf)
    P.add("dve", lambda h: h.tensor_copy(ident_b, ident_f), reads=[B_identf], writes=[B_const])
    P.add("dve", lambda h: h.memset(ones_f, 1.0), writes=[B_const])
    P.add("dve", lambda h: h.memset(ones_b, 1.0), writes=[B_const])
    P.add("dve", lambda h: h.memset(nhalf, -0.5), writes=[B_const])
    CONST_END = A.off

    final_stores = []
    try:
        def rstd_ops(v_ap, out_ap, n, bufs_in, buf_out):
            P.add("pool", lambda h: h.tensor_tensor(out_ap, v_ap, nhalf[:, 0:n], ALU.pow),
                  reads=bufs_in + [B_const], writes=[buf_out])

        qkgt = A.f32(256)
        l4 = A.f32(256)
        lamt = A.f32(8)
        sgcol = A.f32(1)
        masks = A.bf(4 * 512)
        kbt = A.f32(NTILE)
        g1col = A.f32(16)
        CONST_END = A.off

        Wq = A.bf(16 * 1024)
        Wk = A.bf(16 * 1024)
        Wv = A.bf(16 * 1024)
        xs = [A.f32(D) for _ in range(2)]
        junk = A.bf(D)
        xn = [A.bf(D) for _ in range(2)]
        xnT = [A.bf(D) for _ in range(2)]
        smalls = [A.f32(64) for _ in range(2)]
        sqk = A.f32(1024)
        tmpk = A.f32(1024)
        kb_ = A.bf(1024)
        qb_ = A.bf(1024)
        kst = A.bf(8 * 512)
        vst = A.bf(8 * 512)
        qst = A.bf(8 * 512)

        B_xs = [Buf(f"xs{i}") for i in range(2)]
        B_junk = Buf("junk")
        B_xn = [Buf(f"xn{i}") for i in range(2)]
        B_xnT = [Buf(f"xnT{i}") for i in range(2)]
        B_Wq, B_Wk, B_Wv = Buf("Wq"), Buf("Wk"), Buf("Wv")
        B_g1col = Buf("g1col")
        B_sm = [Buf(f"sm{i}") for i in range(2)]
        B_sqk = Buf("sqk")
        B_tmpk = Buf("tmpk")
        B_kb = Buf("kb")
        B_qb = Buf("qb")
        B_kst, B_vst, B_qst = Buf("kst"), Buf("vst"), Buf("qst")
        B_misc = Buf("miscAB")

        dma("sp", g1col, g1.ap(), [], [B_g1col], B_g1col)
        dma("sp", qkgt, qkg.ap()[0:1, :].broadcast_to([128, 256]), [], [B_misc], B_misc)
        B_l4 = Buf("l4")
        dma("sp", l4, lam4.ap()[0:1, :].broadcast_to([128, 256]), [], [B_l4], B_l4)
        B_sg = Buf("sg")
        dma("sp", sgcol, sgn.ap(), [], [B_sg], B_sg)
        B_kbt = Buf("kbt")
        dma("sp", kbt, kbias.ap(), [], [B_kbt], B_kbt)
        B_mk = Buf("masks")
        dma("sp", xs[1], masks_in.ap(), [], [B_xs[1]], B_xs[1])
        P.add("dve", lambda h, src=xs[1]: h.tensor_copy(masks, src), reads=[B_xs[1]], writes=[B_mk])
        P.add("dve", lambda h: h.tensor_scalar(qkgt[:, 0:128], qkgt[:, 0:128], 0.125, None, ALU.mult),
              reads=[B_misc], writes=[B_misc])
        B_lam = Buf("lam")
        P.add("dve", lambda h: h.tensor_tensor(l4[:, 0:64], l4[:, 0:64], l4[:, 64:128], ALU.mult),
              reads=[B_l4], writes=[B_l4])
        P.add("dve", lambda h: h.tensor_tensor(l4[:, 128:192], l4[:, 128:192], l4[:, 192:256], ALU.mult),
              reads=[B_l4], writes=[B_l4])
        P.add("dve", lambda h: h.tensor_reduce(lamt[:, 0:1], l4[:, 0:64], AX.X, ALU.add),
              reads=[B_l4], writes=[B_lam])
        P.add("dve", lambda h: h.tensor_reduce(lamt[:, 1:2], l4[:, 128:192], AX.X, ALU.add),
              reads=[B_l4], writes=[B_lam])
        P.add("act", lambda h: h.activation(lamt[:, 2:4], lamt[:, 0:2], AF.Exp), reads=[B_lam], writes=[B_lam])
        P.add("dve", lambda h: h.tensor_tensor(lamt[:, 4:5], lamt[:, 3:4], lamt[:, 2:3], ALU.subtract),
              reads=[B_lam], writes=[B_lam])
        P.add("dve", lambda h: h.tensor_scalar(lamt[:, 5:6], lamt[:, 4:5], -LAM_INIT, None, ALU.add),
              reads=[B_lam], writes=[B_lam])
        neg_lam = lamt[:, 5:6]
        P.add("dve", lambda h: h.tensor_scalar(sgcol, sgcol, 1.0 - LAM_INIT, None, ALU.mult),
              reads=[B_sg], writes=[B_sg])

        wcnt = [0]

        def load_w1024(src, Wt, B_Wt):
            srcv = src.ap().rearrange("(k p) n -> p k n", p=128)
            Wtv = Wt.rearrange("p (k n) -> p k n", k=16)
            for k2 in range(8):
                s = wcnt[0] % 2
                wcnt[0] += 1
                dma("sp", xs[s].rearrange("p (k n) -> p k n", k=2), srcv[:, 2 * k2:2 * k2 + 2, :], [], [B_xs[s]], B_xs[s])
                dstv = Wtv[:, 2 * k2:2 * k2 + 2, :]
                srcs = xs[s].rearrange("p (k n) -> p k n", k=2)
                if k2 % 2 == 0:
                    P.add("act", lambda h, dstv=dstv, srcs=srcs: h.copy(dstv, srcs), reads=[B_xs[s]], writes=[B_Wt])
                else:
                    P.add("dve", lambda h, dstv=dstv, srcs=srcs: h.tensor_copy(dstv, srcs), reads=[B_xs[s]], writes=[B_Wt])

        load_w1024(w_k, Wk, B_Wk)
        load_w1024(w_v, Wv, B_Wv)
        load_w1024(w_q, Wq, B_Wq)

        def norm_transpose(x_dram_rows, xs_s, Bxs, xn_s, Bxn, sm, Bsm, jk, Bjk, gcol, B_gcol,
                           ps_a, ps_b, out_views, out_bufs):
            dma("sp", xs_s, x_dram_rows, [], [Bxs], Bxs)
            P.add("act", lambda h: h.activation(jk, xs_s, AF.Square, accum_out=sm[:, 0:1]),
                  reads=[Bxs], writes=[Bjk, Bsm])
            P.add("dve", lambda h: h.tensor_scalar(sm[:, 1:2], sm[:, 0:1], 1.0 / D, EPS, ALU.mult, ALU.add),
                  reads=[Bsm], writes=[Bsm])
            rstd_ops(sm[:, 1:2], sm[:, 2:3], 1, [Bsm], Bsm)
            P.add("act", lambda h: h.activation(xn_s, xs_s, AF.Copy, scale=sm[:, 2:3]),
                  reads=[Bxs, Bsm], writes=[Bxn])
            tpa = psb[ps_a].bitcast(BF16)
            tpb = psb[ps_b].bitcast(BF16)
            for kc in range(16):
                tp = (tpa if kc < 8 else tpb)[:, (kc % 8) * 128:(kc % 8 + 1) * 128]
                P.add("pe", lambda h, tp=tp, kc=kc: h.transpose(tp, xn_s[:, kc * 128:(kc + 1) * 128], ident_b),
                      reads=[Bxn, B_const], writes=[PS[ps_a if kc < 8 else ps_b]])
            for kc in range(16):
                tp = (tpa if kc < 8 else tpb)[:, (kc % 8) * 128:(kc % 8 + 1) * 128]
                ov = out_views[kc]
                P.add("dve", lambda h, tp=tp, kc=kc, ov=ov: h.tensor_scalar(ov, tp, gcol[:, kc:kc + 1], None, ALU.mult),
                      reads=[PS[ps_a if kc < 8 else ps_b], B_gcol], writes=out_bufs)

        def a_norm(ti):
            s = ti % 2
            xv = xnT[s].rearrange("p (k t) -> p k t", k=16)
            norm_transpose(x_keys.ap()[ti * 128:(ti + 1) * 128, :], xs[s], B_xs[s], xn[s], B_xn[s],
                           smalls[s], B_sm[s], junk, B_junk, g1col, B_g1col,
                           0, 1, [xv[:, kc, :] for kc in range(16)], [B_xnT[s]])

        def proj(ti, Wt, B_Wt, b0):
            s = ti % 2
            xv = xnT[s].rearrange("p (k t) -> p k t", k=16)
            Wtv = Wt.rearrange("p (k n) -> p k n", k=16)
            for half in range(2):
                for kc in range(16):
                    P.add("pe", lambda h, kc=kc, xv=xv, half=half, Wtv=Wtv: h.matmul(
                        psb[b0 + half], xv[:, kc, :], Wtv[:, kc, half * 512:(half + 1) * 512],
                        start=(kc == 0), stop=(kc == 15)),
                        reads=[B_xnT[s], B_Wt], writes=[PS[b0 + half]])

        def qk_norm(ti, b0, goff, dst, B_dst):
            s = ti % 2
            sm = smalls[s]
            for half in range(2):
                P.add("act", lambda h, half=half: h.activation(sqk[:, half * 512:(half + 1) * 512], psb[b0 + half], AF.Square),
                      reads=[PS[b0 + half]], writes=[B_sqk])
                P.add("dve", lambda h, half=half, sm=sm: h.tensor_reduce(
                    sm[:, 4 + 8 * half:12 + 8 * half], sqk[:, half * 512:(half + 1) * 512].rearrange("p (g d) -> p g d", g=8),
                    AX.X, ALU.add), reads=[B_sqk], writes=[B_sm[s]])
            P.add("dve", lambda h, sm=sm: h.tensor_scalar(sm[:, 20:36], sm[:, 4:20], 1.0 / 64, EPS, ALU.mult, ALU.add),
                  reads=[B_sm[s]], writes=[B_sm[s]])
            rstd_ops(sm[:, 20:36], sm[:, 36:52], 16, [B_sm[s]], B_sm[s])
            for half in range(2):
                P.add("dve", lambda h, half=half, sm=sm: h.tensor_tensor(
                    tmpk[:, half * 512:(half + 1) * 512].rearrange("p (g d) -> p g d", g=8),
                    psb[b0 + half].rearrange("p (g d) -> p g d", g=8),
                    sm[:, 36 + 8 * half:44 + 8 * half].unsqueeze(2).broadcast_to([128, 8, 64]), ALU.mult),
                    reads=[PS[b0 + half], B_sm[s]], writes=[B_tmpk])
                P.add("dve", lambda h, half=half: h.tensor_tensor(
                    dst[:, half * 512:(half + 1) * 512].rearrange("p (g f) -> p g f", g=4),
                    tmpk[:, half * 512:(half + 1) * 512].rearrange("p (g f) -> p g f", g=4),
                    qkgt[:, goff:goff + 128].unsqueeze(1).broadcast_to([128, 4, 128]), ALU.mult),
                    reads=[B_tmpk, B_misc], writes=[B_dst])

        def head_transposes(src, B_src, bank, stage, B_stage, tl):
            tq = psb[bank].bitcast(BF16)
            for hh in range(8):
                P.add("pe", lambda h, hh=hh, tq=tq: h.transpose(tq[:, hh * 128:(hh + 1) * 128], src[:, hh * 128:(hh + 1) * 128], ident_b),
                      reads=[B_src, B_const], writes=[PS[bank]])
            sv = stage.rearrange("p (g t) -> p g t", g=8)[:, :, tl * 128:(tl + 1) * 128]
            P.add("act", lambda h, tq=tq, sv=sv: h.copy(sv, tq.rearrange("p (g t) -> p g t", g=8)),
                  reads=[PS[bank]], writes=[B_stage])

        if stop_after == "A0":
            raise _Stop()
        NQT = NT // 128
        a_norm(0)
        for ti in range(NTILE):
            tl = ti % 4
            proj(ti, Wk, B_Wk, 2)
            if ti + 1 < NTILE:
                a_norm(ti + 1)
            qk_norm(ti, 2, 128, kb_, B_kb)
            proj(ti, Wv, B_Wv, 4)
            vsv = vst.rearrange("p (g t d) -> p g t d", g=8, t=4)
            for half in range(2):
                P.add("act", lambda h, half=half, tl=tl, vsv=vsv: h.copy(
                    vsv[:, 4 * half:4 * half + 4, tl, :], psb[4 + half].rearrange("p (g d) -> p g d", g=4)),
                    reads=[PS[4 + half]], writes=[B_vst])
            if ti < NQT:
                proj(ti, Wq, B_Wq, 2)
                qk_norm(ti, 2, 0, qb_, B_qb)
            head_transposes(kb_, B_kb, 6, kst, B_kst, tl)
            if ti < NQT:
                head_transposes(qb_, B_qb, 7, qst, B_qst, tl)
            if tl == 3:
                t0 = (ti - 3) * 128
                dma("sp", k_scr.ap()[:, :, t0:t0 + 512].rearrange("g p t -> p g t"), kst.rearrange("p (g t) -> p g t", g=8),
                    [B_kst], [Buf(f"kscr{ti}")], B_kst)
                dma("sp", v_scr.ap()[:, :, t0:t0 + 512].rearrange("g p t -> p g t"), vst.rearrange("p (g t) -> p g t", g=8),
                    [B_vst], [Buf(f"vscr{ti}")], B_vst)
                if ti < NQT:
                    dma("sp", q_scr.ap()[:, :, t0:t0 + 512].rearrange("g p t -> p g t"), qst.rearrange("p (g t) -> p g t", g=8),
                        [B_qst], [Buf(f"qscr{ti}")], B_qst)
        P.barrier()

        if stop_after == "A":
            raise _Stop()
        A.off = CONST_END
        kTh = [A.bf(S) for _ in range(2)]
        Vh = [A.bf(S) for _ in range(2)]
        qTh = [A.bf(NT) for _ in range(2)]
        pT = [A.bf(512) for _ in range(4)]
        dacc = [A.f32(512) for _ in range(2)]
        rd = [A.f32(512) for _ in range(2)]
        osb = [A.f32(512) for _ in range(3)]
        sqo = A.f32(512)
        vrs = A.f32(512)
        rs = A.f32(512)
        yb = [A.bf(512) for _ in range(2)]
        B_kTh = [Buf("kTh0"), Buf("kTh1")]
        B_Vh = [Buf("Vh0"), Buf("Vh1")]
        B_qTh = [Buf("qTh0"), Buf("qTh1")]
        B_pT = [Buf(f"pT{i}") for i in range(4)]
        B_dacc = [Buf("dacc0"), Buf("dacc1")]
        B_rd = [Buf(f"rd{i}") for i in range(2)]
        B_os = [Buf(f"os{i}") for i in range(3)]
        B_sqo = Buf("sqo")
        B_vrs = Buf("vrs")
        B_rs = Buf("rs")
        B_yb = [Buf(f"yb{i}") for i in range(2)]

        def load_head(hd):
            s = hd % 2
            dma("sp", kTh[s], k_scr.ap()[hd], [], [B_kTh[s]], B_kTh[s])
            dma("sp", Vh[s], v_scr.ap()[hd], [], [B_Vh[s]], B_Vh[s])
            dma("sp", qTh[s], q_scr.ap()[hd], [], [B_qTh[s]], B_qTh[s])

        cnt = 0
        load_head(0)
        for hd in range(8):
            hs = hd % 2
            if hd + 1 < 8:
                load_head(hd + 1)
            kT, Vt, qT = kTh[hs], Vh[hs], qTh[hs]
            B_kT, B_V, B_qT = B_kTh[hs], B_Vh[hs], B_qTh[hs]
            for qt in range(NT // 512):
                ktiles = list(range(4 * (qt + 1))) + list(range(NQT, NTILE))
                nk = len(ktiles)
                LOOK = 2

                def emit_qk(ki, kt, r, sb, pb):
                    P.add("pe", lambda h, sb=sb, r=r, kt=kt, qt=qt, kT=kT, qT=qT: h.matmul(
                        psb[sb], kT[64 * r:64 * r + 64, kt * 128:(kt + 1) * 128],
                        qT[64 * r:64 * r + 64, qt * 512:(qt + 1) * 512], start=True, stop=True),
                        reads=[B_kT, B_qT], writes=[PS[sb]])
                    if kt < NQT:
                        P.add("act", lambda h, sb=sb, pb=pb: h.activation(pT[pb], psb[sb], AF.Exp),
                              reads=[PS[sb]], writes=[B_pT[pb]])
                        if kt >= 4 * qt:
                            j = kt - 4 * qt
                            P.add("pool", lambda h, pb=pb, j=j: h.tensor_tensor(pT[pb], pT[pb], masks[:, j * 512:(j + 1) * 512], ALU.mult),
                                  reads=[B_pT[pb], B_mk], writes=[B_pT[pb]])
                    else:
                        P.add("act", lambda h, sb=sb, pb=pb, kt=kt: h.activation(pT[pb], psb[sb], AF.Exp, bias=kbt[:, kt:kt + 1]),
                              reads=[PS[sb], B_kbt], writes=[B_pT[pb]])

                def emit_pv(ki, kt, r, sb, pb):
                    P.add("pe", lambda h, pb=pb, r=r, kt=kt, ki=ki, nk=nk, Vt=Vt: h.matmul(
                        psb[3 + r], Vt[:, kt * 128:(kt + 1) * 128], pT[pb], start=(ki == 0), stop=(ki == nk - 1)),
                        reads=[B_V, B_pT[pb]], writes=[PS[3 + r]])
                    deng = "dve" if r == 0 else "pool"
                    if ki == 0:
                        P.add(deng, lambda h, pb=pb, r=r: h.tensor_copy(dacc[r], pT[pb]),
                              reads=[B_pT[pb]], writes=[B_dacc[r]])
                    else:
                        P.add(deng, lambda h, pb=pb, r=r: h.tensor_tensor(dacc[r], dacc[r], pT[pb], ALU.add),
                              reads=[B_pT[pb], B_dacc[r]], writes=[B_dacc[r]])

                pend = []
                for ki, kt in enumerate(ktiles):
                    for r in range(2):
                        it = (ki, kt, r, cnt % 3, cnt % 4)
                        cnt += 1
                        emit_qk(*it)
                        pend.append(it)
                        if len(pend) > LOOK:
                            emit_pv(*pend.pop(0))
                while pend:
                    emit_pv(*pend.pop(0))
                for r in range(2):
                    P.add("pe", lambda h, r=r: h.matmul(psb[5 + r], ones_f, dacc[r], start=True, stop=True),
                          reads=[B_const, B_dacc[r]], writes=[PS[5 + r]])
                    P.add("dve", lambda h, r=r: h.reciprocal(rd[r], psb[5 + r]), reads=[PS[5 + r]], writes=[B_rd[r]])
                    P.add("dve", lambda h, r=r: h.tensor_tensor(osb[r], psb[3 + r], rd[r], ALU.mult),
                          reads=[PS[3 + r], B_rd[r]], writes=[B_os[r]])
                P.add("dve", lambda h: h.scalar_tensor_tensor(osb[2], osb[1], neg_lam, osb[0], ALU.mult, ALU.add),
                      reads=[B_os[0], B_os[1], B_lam], writes=[B_os[2]])
                P.add("act", lambda h: h.activation(sqo, osb[2], AF.Square), reads=[B_os[2]], writes=[B_sqo])
                P.add("pe", lambda h: h.matmul(psb[7], ones_f, sqo, start=True, stop=True),
                      reads=[B_const, B_sqo], writes=[PS[7]])
                P.add("dve", lambda h: h.tensor_scalar(vrs, psb[7], 1.0 / 128, EPS, ALU.mult, ALU.add),
                      reads=[PS[7]], writes=[B_vrs])
                rstd_ops(vrs, rs, 512, [B_vrs], B_rs)
                P.add("dve", lambda h: h.tensor_tensor(osb[2], osb[2], rs, ALU.mult),
                      reads=[B_os[2], B_rs], writes=[B_os[2]])
                ys = qt % 2
                P.add("act", lambda h, ys=ys: h.activation(yb[ys], osb[2], AF.Copy, scale=sgcol),
                      reads=[B_os[2], B_sg], writes=[B_yb[ys]])
                dma("sp", ya_scr.ap()[hd * 128:(hd + 1) * 128, qt * 512:(qt + 1) * 512], yb[ys], [B_yb[ys]],
                    [Buf(f"yascr{hd}_{qt}")], B_yb[ys])
        P.barrier()

        if stop_after == "B":
            raise _Stop()
        A.off = CONST_END
        W64 = A.bf(16 * 2048)
        xs2 = [A.f32(D) for _ in range(2)]
        junk2 = A.bf(D)
        xn2 = [A.bf(D) for _ in range(2)]
        smalls2 = [A.f32(16) for _ in range(2)]
        _r1 = A.off
        xnTg = A.bf(16 * 512)
        hTt = A.f32(8 * 544)
        _r1_end = A.off
        accs = A.f32(8 * 512)
        ycT = A.bf(8 * NT)
        assert _r1_end - _r1 >= 8 * NT * 2
        yaT = arena[:, _r1 // 2:_r1 // 2 + 8 * NT]
        sig = A.f32(512)
        sqc = A.f32(512)
        mean = A.f32(512)
        msq = A.f32(512)
        var = A.f32(512)
        rstdc = A.f32(512)
        zt = A.f32(512)
        cwT = A.f32(8 * 31)
        cbc = A.f32(8)
        lngc = A.f32(8)
        lnbc = A.f32(8)
        g1c2 = A.f32(16)
        D_END = A.off

        xs[0], xs[1] = xs2
        xn[0], xn[1] = xn2
        smalls[0], smalls[1] = smalls2
        junk = junk2
        B_xs = [Buf("xs2a"), Buf("xs2b")]
        B_xn = [Buf("xn2a"), Buf("xn2b")]
        B_sm = [Buf("sm2a"), Buf("sm2b")]
        B_junk = Buf("junk2")

        B_W64 = Buf("W64")
        B_g1c2 = Buf("g1c2")
        B_small = Buf("smallD")
        B_xnTg = Buf("xnTg")
        B_hT = Buf("hT")
        B_accs = [Buf(f"accs{i}") for i in range(8)]
        B_ycT = Buf("ycT")
        B_yaT = Buf("yaT")
        B_sig = Buf("sig")
        B_sqc = Buf("sqc")
        B_stat = Buf("stat")
        B_zt = Buf("zt")

        dma("sp", g1c2, g1.ap(), [], [B_g1c2], B_g1c2)
        dma("sp", cwT.rearrange("p (c j) -> p c j", c=8), cwk.ap().rearrange("(c p) j -> p c j", p=128), [], [B_small], B_small)
        B_s2 = Buf("s2")
        dma("sp", cbc, cbias.ap(), [], [B_s2], B_s2)
        B_s3 = Buf("s3")
        dma("sp", lngc, lng.ap(), [], [B_s3], B_s3)
        B_s4 = Buf("s4")
        dma("sp", lnbc, lnb.ap(), [], [B_s4], B_s4)

        W64v = W64.rearrange("p (k n) -> p k n", k=16)

        def load_w64(src):
            srcv = src.ap().rearrange("(k p) n -> p k n", p=128)
            for kc in range(16):
                s = kc % 2
                dma("sp", xs[s], srcv[:, kc, :], [], [B_xs[s]], B_xs[s])
                eng = "act" if kc % 2 == 0 else "dve"
                if eng == "act":
                    P.add("act", lambda h, s=s, kc=kc: h.copy(W64v[:, kc, :], xs[s]), reads=[B_xs[s]], writes=[B_W64])
                else:
                    P.add("dve", lambda h, s=s, kc=kc: h.tensor_copy(W64v[:, kc, :], xs[s]), reads=[B_xs[s]], writes=[B_W64])

        load_w64(w_conv)
        hTv = hTt.rearrange("p (c t) -> p c t", c=8)
        accv = accs.rearrange("p (c t) -> p c t", c=8)
        ycv = ycT.rearrange("p (c t) -> p c t", c=8)
        yav = yaT.rearrange("p (c t) -> p c t", c=8)
        xgv = xnTg.rearrange("p (k t) -> p k t", k=16)
        cwv = cwT.rearrange("p (c j) -> p c j", c=8)
        P.add("dve", lambda h: h.memset(hTt, 0.0), writes=[B_hT])

        groups = [(0, 128, True)] + [(HALO + 512 * j, 512, False) for j in range(NGRP)]
        for gi, (r0, ntok, is_halo) in enumerate(groups):
            for t in range(ntok // 128):
                norm_transpose(x_own.ap()[r0 + t * 128:r0 + (t + 1) * 128, :], xs[t % 2], B_xs[t % 2],
                               xn[t % 2], B_xn[t % 2], smalls[t % 2], B_sm[t % 2], junk, B_junk, g1c2, B_g1c2, 0, 1,
                               [xgv[:, kc, t * 128:(t + 1) * 128] for kc in range(16)], [B_xnTg])
            if gi >= 1:
                pn = groups[gi - 1][1]
                for c in range(8):
                    P.add("dve", lambda h, c=c, pn=pn: h.tensor_copy(hTv[:, c, 2:32], hTv[:, c, 32 + pn - 30:32 + pn]),
                          reads=[B_hT], writes=[B_hT])
            for c in range(8):
                for half in range(2):
                    pb_ = 2 + half
                    cofs = half * 1024 + c * 128
                    for kc in range(16):
                        P.add("pe", lambda h, pb_=pb_, kc=kc, cofs=cofs, ntok=ntok: h.matmul(
                            psb[pb_][:, 0:ntok], W64v[:, kc, cofs:cofs + 128], xgv[:, kc, 0:ntok],
                            start=(kc == 0), stop=(kc == 15)),
                            reads=[B_W64, B_xnTg], writes=[PS[pb_]])
                P.add("act", lambda h, ntok=ntok: h.activation(sig[:, 0:ntok], psb[3][:, 0:ntok], AF.Sigmoid),
                      reads=[PS[3]], writes=[B_sig])
                P.add("dve", lambda h, c=c, ntok=ntok: h.tensor_tensor(hTv[:, c, 32:32 + ntok], psb[2][:, 0:ntok], sig[:, 0:ntok], ALU.mult),
                      reads=[PS[2], B_sig], writes=[B_hT])
            if is_halo:
                continue
            j = gi - 1
            for c in range(8):
                eng = "dve"
                P.add(eng, lambda h, c=c: h.tensor_scalar(accv[:, c, :], hTv[:, c, 2:2 + 512], cwv[:, c, 0:1], cbc[:, c:c + 1], ALU.mult, ALU.add),
                      reads=[B_hT, B_small, B_s2], writes=[B_accs[c]])
                for k in range(1, 31):
                    P.add(eng, lambda h, c=c, k=k: h.scalar_tensor_tensor(accv[:, c, :], hTv[:, c, 2 + k:2 + k + 512], cwv[:, c, k:k + 1], accv[:, c, :], ALU.mult, ALU.add),
                          reads=[B_hT, B_small, B_accs[c]], writes=[B_accs[c]])
            for c in range(8):
                P.add("act", lambda h, c=c: h.activation(sqc, accv[:, c, :], AF.Square), reads=[B_accs[c]], writes=[B_sqc])
                P.add("pe", lambda h, c=c: h.matmul(psb[4], ones_f, accv[:, c, :], start=(c == 0), stop=(c == 7)),
                      reads=[B_const, B_accs[c]], writes=[PS[4]])
                P.add("pe", lambda h, c=c: h.matmul(psb[5], ones_f, sqc, start=(c == 0), stop=(c == 7)),
                      reads=[B_const, B_sqc], writes=[PS[5]])
            P.add("dve", lambda h: h.tensor_scalar(mean, psb[4], 1.0 / 1024, None, ALU.mult), reads=[PS[4]], writes=[B_stat])
            P.add("dve", lambda h: h.tensor_tensor(msq, mean, mean, ALU.mult), reads=[B_stat], writes=[B_stat])
            P.add("dve", lambda h: h.scalar_tensor_tensor(var, psb[5], 1.0 / 1024, msq, ALU.mult, ALU.subtract),
                  reads=[PS[5], B_stat], writes=[B_stat])
            P.add("dve", lambda h: h.tensor_scalar(var, var, EPS, None, ALU.add), reads=[B_stat], writes=[B_stat])
            rstd_ops(var, rstdc, 512, [B_stat], B_stat)
            for c in range(8):
                P.add("dve", lambda h, c=c: h.tensor_tensor(zt, accv[:, c, :], mean, ALU.subtract),
                      reads=[B_accs[c], B_stat], writes=[B_zt])
                P.add("dve", lambda h: h.tensor_tensor(zt, zt, rstdc, ALU.mult), reads=[B_zt, B_stat], writes=[B_zt])
                P.add("act", lambda h, c=c, j=j: h.activation(ycv[:, c, j * 512:(j + 1) * 512], zt, AF.Silu,
                                                            bias=lnbc[:, c:c + 1], scale=lngc[:, c:c + 1]),
                      reads=[B_zt, B_s3, B_s4], writes=[B_ycT])

        if stop_after == "D1":
            raise _Stop()
        P.barrier()
        for hh in range(8):
            dma("sp", yav[:, hh, :], ya_scr.ap()[hh * 128:(hh + 1) * 128, :], [], [B_yaT], B_yaT)

        load_w64(w_out)
        B_hs = [Buf("hs0"), Buf("hs1")]
        for tt in range(NT // 128):
            s = tt % 2
            dma("sp", xs[s], x_own.ap()[HALO + tt * 128:HALO + (tt + 1) * 128, :], [], [B_xs[s]], B_xs[s])
            for cg in range(4):
                pbk = 4 + cg
                for kc in range(16):
                    src = ycv[:, kc, tt * 128:(tt + 1) * 128] if kc < 8 else yav[:, kc - 8, tt * 128:(tt + 1) * 128]
                    P.add("pe", lambda h, pbk=pbk, kc=kc, src=src, cg=cg: h.matmul(
                        psb[pbk], src, W64v[:, kc, cg * 512:(cg + 1) * 512], start=(kc == 0), stop=(kc == 15)),
                        reads=[B_ycT, B_yaT, B_W64], writes=[PS[pbk]])
                P.add("dve", lambda h, pbk=pbk, s=s, cg=cg: h.tensor_tensor(xs[s][:, cg * 512:(cg + 1) * 512], xs[s][:, cg * 512:(cg + 1) * 512], psb[pbk], ALU.add),
                      reads=[PS[pbk], B_xs[s]], writes=[B_xs[s]])
            B_hrow = Buf(f"hrow{tt}")
            dma("sp", h_scr.ap()[tt * 128:(tt + 1) * 128, :], xs[s], [B_xs[s]], [B_hrow], B_xs[s])
        P.barrier()

        if stop_after == "D":
            raise _Stop()
        A.off = CONST_END
        NPASS = NT // 512
        TPP = 4
        acc = [A.f32(D) for _ in range(TPP)]
        tT = A.bf(16 * 512)
        Wsl = [A.bf(16 * 512) for _ in range(4)]
        st = [A.f32(D) for _ in range(3)]
        hTm = A.bf(4 * 512)
        junk3 = A.bf(D)
        tn32 = A.f32(D)
        hib = A.bf(D)
        lob = A.bf(D)
        thT = A.bf(D)
        tlT = A.bf(D)
        Wg32 = A.f32(16 * 36)
        Whb = A.bf(16 * 36)
        Wlb = A.bf(16 * 36)
        sgl = [A.f32(512) for _ in range(2)]
        cwt = A.f32(TPP * 32)
        Wr32 = A.f32(16 * 36)
        brt = A.f32(36)
        g2col = A.f32(16)
        lg = A.f32(36)
        rt = A.f32(64)
        sm3 = A.f32(8)

        B_acc = [Buf(f"acc{i}") for i in range(TPP)]
        B_tT = Buf("tT")
        B_W = [Buf(f"Wsl{i}") for i in range(4)]
        B_st = [Buf(f"st{i}") for i in range(3)]
        B_hTm = Buf("hTm")
        B_junk3 = Buf("junk3")
        B_tn32 = Buf("tn32")
        B_hib, B_lob, B_thT, B_tlT = Buf("hib"), Buf("lob"), Buf("thT"), Buf("tlT")
        B_sgl = [Buf("sgl0"), Buf("sgl1")]
        B_cwt = Buf("cwt")
        B_Wr = Buf("Wr32")
        B_brt = Buf("brt")
        B_g2 = Buf("g2col")
        B_lg = Buf("lg")
        B_rt = Buf("rt")
        B_sm3 = Buf("sm3")

        dma("sp", Wr32, w_rt.ap(), [], [B_Wr], B_Wr)
        dma("sp", brt, b_rt.ap()[0:1, :].broadcast_to([128, 36]), [], [B_brt], B_brt)
        dma("sp", g2col, g2.ap(), [], [B_g2], B_g2)
        if stop_after == "E0a":
            raise _Stop()
        Wrv = Wr32.rearrange("p (k n) -> p k n", k=16)
        tTv = tT.rearrange("p (k t) -> p k t", k=16)
        P.add("dve", lambda h: h.tensor_tensor(Wg32.rearrange("p (k n) -> p k n", k=16), Wrv,
                                               g2col.unsqueeze(2).broadcast_to([128, 16, 36]), ALU.mult),
              reads=[B_Wr, B_g2], writes=[B_Wr])
        P.add("act", lambda h: h.copy(Whb, Wg32), reads=[B_Wr], writes=[B_Wr])
        P.add("dve", lambda h: h.tensor_tensor(Wlb, Wg32, Whb, ALU.subtract), reads=[B_Wr], writes=[B_Wr])
        Whv = Whb.rearrange("p (k n) -> p k n", k=16)
        Wlv = Wlb.rearrange("p (k n) -> p k n", k=16)
        hmv = hTm.rearrange("p (f t) -> p f t", f=4)
        cwtv = cwt.rearrange("p (t e) -> p t e", t=TPP)
        wq = [0]
        sq_ = [0]
        final_stores = []

        def load_expert_matrix(src_view, npieces, piece_view_fn):
            slot = wq[0] % 4
            wq[0] += 1
            for pc in range(npieces):
                s = sq_[0] % 3
                sq_[0] += 1
                dma("sp", piece_view_fn(st[s]), src_view(pc), [], [B_st[s]], B_st[s])
                dst = Wsl[slot][:, pc * 2048:(pc + 1) * 2048]
                if pc % 2 == 0:
                    P.add("act", lambda h, s=s, dst=dst: h.copy(dst, st[s]), reads=[B_st[s]], writes=[B_W[slot]])
                else:
                    P.add("pool", lambda h, s=s, dst=dst: h.tensor_copy(dst, st[s]), reads=[B_st[s]], writes=[B_W[slot]])
            return slot

        for ps_ in range(NPASS):
            for tt in range(TPP):
                row0 = (ps_ * TPP + tt) * 128
                dma("sp", acc[tt], h_scr.ap()[row0:row0 + 128, :], [], [B_acc[tt]], B_acc[tt])
                P.add("act", lambda h, tt=tt: h.activation(junk3, acc[tt], AF.Square, accum_out=sm3[:, 0:1]),
                      reads=[B_acc[tt]], writes=[B_junk3, B_sm3])
                P.add("dve", lambda h: h.tensor_scalar(sm3[:, 1:2], sm3[:, 0:1], 1.0 / D, EPS, ALU.mult, ALU.add),
                      reads=[B_sm3], writes=[B_sm3])
                rstd_ops(sm3[:, 1:2], sm3[:, 2:3], 1, [B_sm3], B_sm3)
                P.add("act", lambda h, tt=tt: h.activation(tn32, acc[tt], AF.Copy, scale=sm3[:, 2:3]),
                      reads=[B_acc[tt], B_sm3], writes=[B_tn32])
                if stop_after == "E0b":
                    raise _Stop()
                P.add("act", lambda h: h.copy(hib, tn32), reads=[B_tn32], writes=[B_hib])
                P.add("dve", lambda h: h.tensor_tensor(lob, tn32, hib, ALU.subtract), reads=[B_tn32, B_hib], writes=[B_lob])
                for (srcb, B_srcb, bk) in ((hib, B_hib, 0), (lob, B_lob, 2)):
                    for kc in range(16):
                        tpv = psb[bk + kc // 8].bitcast(BF16)[:, (kc % 8) * 128:(kc % 8 + 1) * 128]
                        P.add("pe", lambda h, kc=kc, tpv=tpv, srcb=srcb: h.transpose(tpv, srcb[:, kc * 128:(kc + 1) * 128], ident_b),
                              reads=[B_srcb, B_const], writes=[PS[bk + kc // 8]])
                for hf in range(2):
                    P.add("dve", lambda h, hf=hf: h.tensor_copy(thT[:, hf * 1024:(hf + 1) * 1024], psb[0 + hf].bitcast(BF16)),
                          reads=[PS[0 + hf]], writes=[B_thT])
                    P.add("act", lambda h, hf=hf: h.copy(tlT[:, hf * 1024:(hf + 1) * 1024], psb[2 + hf].bitcast(BF16)),
                          reads=[PS[2 + hf]], writes=[B_tlT])
                P.add("pool", lambda h, tt=tt: h.tensor_tensor(tTv[:, :, tt * 128:(tt + 1) * 128], thT.rearrange("p (k t) -> p k t", k=16),
                                                             g2col.unsqueeze(2).broadcast_to([128, 16, 128]), ALU.mult),
                      reads=[B_thT, B_g2], writes=[B_tT])
                if stop_after == "E1a":
                    raise _Stop()
                thv = thT.rearrange("p (k t) -> p k t", k=16)
                tlv = tlT.rearrange("p (k t) -> p k t", k=16)
                combos = [(thv, B_thT, Whv), (thv, B_thT, Wlv), (tlv, B_tlT, Whv), (tlv, B_tlT, Wlv)]
                for ci, (tv_, B_tv, wv_) in enumerate(combos):
                    for kc in range(16):
                        P.add("pe", lambda h, kc=kc, tv_=tv_, wv_=wv_, ci=ci: h.matmul(
                            psb[4][:, 0:36], tv_[:, kc, :], wv_[:, kc, :], start=(ci == 0 and kc == 0), stop=(ci == 3 and kc == 15)),
                            reads=[B_tv, B_Wr], writes=[PS[4]])
                P.add("dve", lambda h: h.tensor_tensor(lg, psb[4][:, 0:36], brt, ALU.add), reads=[PS[4], B_brt], writes=[B_lg])
                if stop_after == "E1b":
                    raise _Stop()
                R = rt
                def dv(fn, reads, writes):
                    P.add("dve", fn, reads=reads, writes=writes)
                dv(lambda h: h.tensor_reduce(R[:, 0:1], lg[:, 0:4], AX.X, ALU.max), [B_lg], [B_rt])
                dv(lambda h: h.tensor_scalar(R[:, 1:2], R[:, 0:1], -1.0, None, ALU.mult), [B_rt], [B_rt])
                P.add("act", lambda h: h.activation(R[:, 56:60], lg[:, 0:4], AF.Exp, bias=R[:, 1:2], accum_out=R[:, 2:3]),
                      reads=[B_lg, B_rt], writes=[B_rt])
                dv(lambda h: h.reciprocal(R[:, 3:4], R[:, 2:3]), [B_rt], [B_rt])
                dv(lambda h: h.tensor_scalar(R[:, 4:8], lg[:, 0:4], R[:, 0:1], None, ALU.is_equal), [B_lg, B_rt], [B_rt])
                dv(lambda h: h.tensor_scalar(R[:, 8:16], lg[:, 4:12], R[:, 4:5], None, ALU.mult), [B_lg, B_rt], [B_rt])
                for g in range(1, 4):
                    dv(lambda h, g=g: h.scalar_tensor_tensor(R[:, 8:16], lg[:, 4 + 8 * g:12 + 8 * g], R[:, 4 + g:5 + g], R[:, 8:16], ALU.mult, ALU.add),
                       [B_lg, B_rt], [B_rt])
                dv(lambda h: h.tensor_reduce(R[:, 16:17], R[:, 8:16], AX.X, ALU.max), [B_rt], [B_rt])
                dv(lambda h: h.tensor_scalar(R[:, 17:25], R[:, 8:16], R[:, 16:17], None, ALU.is_equal), [B_rt], [B_rt])
                dv(lambda h: h.scalar_tensor_tensor(R[:, 25:33], R[:, 17:25], -1.0e30, R[:, 8:16], ALU.mult, ALU.add), [B_rt], [B_rt])
                dv(lambda h: h.tensor_reduce(R[:, 33:34], R[:, 25:33], AX.X, ALU.max), [B_rt], [B_rt])
                dv(lambda h: h.tensor_scalar(R[:, 34:42], R[:, 25:33], R[:, 33:34], None, ALU.is_equal), [B_rt], [B_rt])
                dv(lambda h: h.tensor_tensor(R[:, 42:43], R[:, 33:34], R[:, 16:17], ALU.subtract), [B_rt], [B_rt])
                P.add("act", lambda h: h.activation(R[:, 43:44], R[:, 42:43], AF.Exp), reads=[B_rt], writes=[B_rt])
                dv(lambda h: h.tensor_scalar(R[:, 44:45], R[:, 43:44], 1.0, None, ALU.add), [B_rt], [B_rt])
                dv(lambda h: h.reciprocal(R[:, 45:46], R[:, 44:45]), [B_rt], [B_rt])
                dv(lambda h: h.tensor_tensor(R[:, 46:47], R[:, 43:44], R[:, 45:46], ALU.mult), [B_rt], [B_rt])
                dv(lambda h: h.tensor_scalar(R[:, 48:56], R[:, 17:25], R[:, 45:46], None, ALU.mult), [B_rt], [B_rt])
                dv(lambda h: h.scalar_tensor_tensor(R[:, 48:56], R[:, 34:42], R[:, 46:47], R[:, 48:56], ALU.mult, ALU.add), [B_rt], [B_rt])
                dv(lambda h: h.tensor_scalar(R[:, 48:56], R[:, 48:56], R[:, 3:4], None, ALU.mult), [B_rt], [B_rt])
                for g in range(4):
                    dv(lambda h, g=g, tt=tt: h.tensor_scalar(cwtv[:, tt, 8 * g:8 * g + 8], R[:, 48:56], R[:, 4 + g:5 + g], None, ALU.mult),
                       [B_rt], [B_cwt])
            if stop_after == "E1":
                raise _Stop()
            for e in range(NEXP_RUN):
                gsl = load_expert_matrix(lambda pc, e=e: w_gate.ap()[e].rearrange("(k p) f -> p k f", p=128)[:, 4 * pc:4 * pc + 4, :],
                                         4, lambda stt: stt.rearrange("p (k f) -> p k f", k=4))
                usl = load_expert_matrix(lambda pc, e=e: w_up.ap()[e].rearrange("(k p) f -> p k f", p=128)[:, 4 * pc:4 * pc + 4, :],
                                         4, lambda stt: stt.rearrange("p (k f) -> p k f", k=4))
                dsl = load_expert_matrix(lambda pc, e=e: w_down.ap()[e][pc * 128:(pc + 1) * 128, :],
                                         4, lambda stt: stt)
                Wg = Wsl[gsl].rearrange("p (k f) -> p k f", k=16)
                Wu = Wsl[usl].rearrange("p (k f) -> p k f", k=16)
                Wd = Wsl[dsl].rearrange("p (f n) -> p f n", f=4)
                for fc in range(4):
                    gb = 0 + (fc % 2)
                    ub = 2 + (fc % 2)
                    for kc in range(16):
                        P.add("pe", lambda h, gb=gb, kc=kc, fc=fc, Wg=Wg: h.matmul(psb[gb], Wg[:, kc, fc * 128:(fc + 1) * 128], tTv[:, kc, :], start=(kc == 0), stop=(kc == 15)),
                              reads=[B_W[gsl], B_tT], writes=[PS[gb]])
                    for kc in range(16):
                        P.add("pe", lambda h, ub=ub, kc=kc, fc=fc, Wu=Wu: h.matmul(psb[ub], Wu[:, kc, fc * 128:(fc + 1) * 128], tTv[:, kc, :], start=(kc == 0), stop=(kc == 15)),
                              reads=[B_W[usl], B_tT], writes=[PS[ub]])
                    sl = fc % 2
                    P.add("act", lambda h, gb=gb, sl=sl: h.activation(sgl[sl], psb[gb], AF.Silu), reads=[PS[gb]], writes=[B_sgl[sl]])
                    P.add("dve", lambda h, ub=ub, sl=sl, fc=fc: h.tensor_tensor(hmv[:, fc, :], sgl[sl], psb[ub], ALU.mult),
                          reads=[B_sgl[sl], PS[ub]], writes=[B_hTm])
                oc = 0
                for tt in range(TPP):
                    for cg in range(4):
                        ob = 4 + (oc % 4)
                        oc += 1
                        for fc in range(4):
                            P.add("pe", lambda h, ob=ob, fc=fc, tt=tt, cg=cg, Wd=Wd: h.matmul(psb[ob], hmv[:, fc, tt * 128:(tt + 1) * 128], Wd[:, fc, cg * 512:(cg + 1) * 512], start=(fc == 0), stop=(fc == 3)),
                                  reads=[B_hTm, B_W[dsl]], writes=[PS[ob]])
                        P.add("dve", lambda h, ob=ob, tt=tt, cg=cg, e=e: h.scalar_tensor_tensor(
                            acc[tt][:, cg * 512:(cg + 1) * 512], psb[ob], cwtv[:, tt, e:e + 1], acc[tt][:, cg * 512:(cg + 1) * 512], ALU.mult, ALU.add),
                            reads=[PS[ob], B_cwt, B_acc[tt]], writes=[B_acc[tt]])
            for tt in range(TPP):
                row0 = (ps_ * TPP + tt) * 128
                B_yrow = Buf(f"yrow{row0}")
                final_stores.append(dma("sp", y_out.ap()[row0:row0 + 128, :], acc[tt], [B_acc[tt]], [B_yrow], B_acc[tt]))

    except _Stop:
        pass

    fin = P.add("sp", lambda h: h.nop(), reads=[], writes=[])
    for so in final_stores:
        fin.deps.append(so)

    P.finalize()
    with nc.Block() as block:
        @block.tensor
        def _(e):
            P.emit("pe", e)

        @block.scalar
        def _(e):
            P.emit("act", e)

        @block.vector
        def _(e):
            P.emit("dve", e)

        @block.gpsimd
        def _(e):
            P.emit("pool", e)

        @block.sync
        def _(e):
            P.emit("sp", e)
    stack.close()
    return nc


def _masks():
    k = np.arange(128)[:, None]
    q = np.arange(512)[None, :]
    m = np.zeros((128, 4 * 512), np.float32)
    for j in range(4):
        m[:, j * 512:(j + 1) * 512] = ((2 * j + k // 64) <= (q // 64)).astype(np.float32)
    return m


def make_in_maps(x, norm1_g, w_in, conv_dw_kernel, conv_dw_bias, conv_ln_g, conv_ln_b,
                 q_norm_g, k_norm_g, lambda_q1, lambda_k1, lambda_q2, lambda_k2, subln_g,
                 w_out, norm2_g, w_group, b_group, w_router, b_router, w_gate, w_up, w_down, nexp_run=NEXP):
    f = lambda a: np.ascontiguousarray(np.asarray(a, dtype=np.float32))
    S = int(np.asarray(x).shape[1])
    NT = S // NCORE
    NOWN = NT + HALO
    NTILE = S // 128
    x2 = f(x).reshape(S, D)
    w_in0 = f(w_in)[0]
    xpad = np.concatenate([np.zeros((HALO, D), np.float32), x2], axis=0)
    w_rt = np.concatenate([f(w_group)[0]] + [f(w_router)[0, g] for g in range(4)], axis=1)
    b_rt = np.concatenate([f(b_group)[0]] + [f(b_router)[0, g] for g in range(4)], axis=0)[None, :]
    common = dict(
        w_conv=f(w_in0[:, 0:2048]), w_q=f(w_in0[:, 2048:3072]), w_k=f(w_in0[:, 3072:4096]),
        w_v=f(w_in0[:, 4096:5120]), g1=f(f(norm1_g).reshape(16, 128).T),
        cwk=f(f(conv_dw_kernel)[0].T), cbias=f(f(conv_dw_bias).reshape(8, 128).T),
        lng=f(f(conv_ln_g).reshape(8, 128).T), lnb=f(f(conv_ln_b).reshape(8, 128).T),
        qkg=np.concatenate([f(q_norm_g).reshape(1, 128), f(k_norm_g).reshape(1, 128)], axis=1),
        lam4=np.concatenate([f(lambda_q1).reshape(1, 64), f(lambda_k1).reshape(1, 64),
                             f(lambda_q2).reshape(1, 64), f(lambda_k2).reshape(1, 64)], axis=1),
        sgn=f(f(subln_g).reshape(1, 128).T), w_out=f(w_out)[0], g2=f(f(norm2_g).reshape(16, 128).T),
        w_rt=f(w_rt.reshape(16, 128, 36).transpose(1, 0, 2).reshape(128, 16 * 36)), b_rt=f(b_rt), w_gate=f(f(w_gate)[0][:nexp_run]), w_up=f(f(w_up)[0][:nexp_run]),
        w_down=f(f(w_down)[0][:nexp_run]),
        ident=np.eye(128, dtype=np.float32), masks=_masks(),
    )
    maps = []
    for c in range(NCORE):
        m = dict(common)
        m["x_own"] = f(xpad[c * NT:c * NT + NOWN])
        order = [c] + [b for b in range(NCORE) if b != c]
        m["x_keys"] = f(np.concatenate([x2[b * NT:(b + 1) * NT] for b in order], axis=0))
        kb = np.zeros((128, NTILE), np.float32)
        for pos, b in enumerate(order):
            if b > c:
                kb[:, pos * (NT // 128):(pos + 1) * (NT // 128)] = -30000.0
        m["kbias"] = kb
        maps.append(m)
    return maps


_NC_CACHE = {}


def kernel(**inputs):
    if "nc" not in _NC_CACHE:
        _NC_CACHE["nc"] = build_program()
    nc = _NC_CACHE["nc"]
    maps = make_in_maps(**inputs)
    res = run_bass_kernel_spmd(nc, maps, core_ids=list(range(NCORE)))
    out = np.concatenate([np.asarray(r["y"], dtype=np.float32) for r in res.results], axis=0)
    return out.reshape(1, S_FULL, D)
```

```python
import numpy as np
import concourse.bass as bass
import concourse.mybir as mybir
from concourse.bass_utils import run_bass_kernel_spmd
from contextlib import ExitStack

F32 = mybir.dt.float32
BF16 = mybir.dt.bfloat16
ALU = mybir.AluOpType
AF = mybir.ActivationFunctionType
AX = mybir.AxisListType

S_FULL = 16384
D = 2048
NCORE = 8
HALO = 128
EPS = 1e-6
NEXP = 32
DEXP = 512
LAM_INIT = 0.2
SEM_LIMIT = 30000


class Buf:
    def __init__(self, name):
        self.name = name
        self.last_w = None
        self.readers = []
        self.chan = None


class Op:
    __slots__ = ("eng", "fn", "deps", "flagged", "sem", "val", "kind", "chan", "idx")

    def __init__(self, eng, fn, kind):
        self.eng = eng
        self.fn = fn
        self.deps = []
        self.flagged = False
        self.sem = None
        self.val = 0
        self.kind = kind
        self.chan = None


class Chan:
    def __init__(self, sem):
        self.sem = sem
        self.count = 0


class Prog:
    ENGS = ("pe", "act", "dve", "pool", "sp")

    def __init__(self, nc, stack):
        self.nc = nc
        self.stack = stack
        self.ops = {e: [] for e in self.ENGS}
        self.nsem = 0
        self.bar_deps = {e: [] for e in self.ENGS}
        self.dma_since_bar = []
        self.free_chans = []
        self.phase_bufs = []

    def new_sem(self, name):
        self.nsem += 1
        return self.stack.enter_context(self.nc.semaphore(f"{name}_{self.nsem}"))

    def add(self, eng, fn, reads=(), writes=(), kind="c", chan_buf=None):
        op = Op(eng, fn, kind)
        deps = []
        if self.bar_deps[eng]:
            deps.extend(self.bar_deps[eng])
            self.bar_deps[eng] = []
        for b in reads:
            if b.last_w is not None:
                deps.append(b.last_w)
        for b in writes:
            if b.last_w is not None:
                deps.append(b.last_w)
            deps.extend(b.readers)
        for b in writes:
            b.last_w = op
            b.readers = []
        for b in reads:
            if b.last_w is op:
                continue
            if kind == "c":
                b.readers = [r for r in b.readers if not (r.kind == "c" and r.eng == eng)]
            b.readers.append(op)
        seen = set()
        for d in deps:
            if d is op or id(d) in seen:
                continue
            seen.add(id(d))
            if d.kind == "c" and d.eng == "pe" and eng == "pe" and kind == "c":
                continue
            op.deps.append(d)
            d.flagged = True
        if kind == "d":
            cb = chan_buf
            if cb.chan is None:
                cb.chan = self.free_chans.pop() if self.free_chans else Chan(self.new_sem("dq"))
                self.phase_bufs.append(cb)
            cb.chan.count += 1
            op.chan = cb.chan
            op.sem = cb.chan.sem
            op.val = cb.chan.count * 16
            self.dma_since_bar.append(op)
        elif kind == "cc":
            op.sem = self.new_sem("cc")
            op.val = 1
        self.ops[eng].append(op)
        return op

    def barrier(self):
        lasts = []
        for e in self.ENGS:
            for op in reversed(self.ops[e]):
                if op.kind != "d":
                    lasts.append(op)
                    op.flagged = True
                    break
        lasts.extend(self.dma_since_bar)
        self.dma_since_bar = []
        for e in self.ENGS:
            self.bar_deps[e] = self.bar_deps[e] + list(lasts)
        for b in self.phase_bufs:
            self.free_chans.append(b.chan)
            b.chan = None
        self.phase_bufs = []

    def finalize(self):
        for e in self.ENGS:
            cnt = 0
            sems = []
            for op in self.ops[e]:
                if op.kind == "c" and op.flagged:
                    si = cnt // SEM_LIMIT
                    if si >= len(sems):
                        sems.append(self.new_sem("e" + e))
                    op.sem = sems[si]
                    op.val = cnt % SEM_LIMIT + 1
                    cnt += 1

    def emit(self, eng, h):
        waited = {}
        for op in self.ops[eng]:
            need = {}
            for d in op.deps:
                if need.get(d.sem, 0) < d.val:
                    need[d.sem] = d.val
            for s, v in need.items():
                if waited.get(s, 0) < v:
                    h.wait_ge(s, v)
                    waited[s] = v
            ins = op.fn(h)
            if op.kind == "d":
                ins.then_inc(op.sem, 16)
            elif op.kind == "cc":
                ins.then_inc(op.sem)
            elif op.flagged:
                ins.then_inc(op.sem, 1)


class _Stop(Exception):
    pass


def build_program(S=S_FULL, NEXP_RUN=NEXP, stop_after=None):
    NT = S // NCORE
    NOWN = NT + HALO
    NGRP = NT // 512
    nc = bass.Bass("TRN2", target_bir_lowering=False)
    stack = ExitStack()
    P = Prog(nc, stack)

    def din(name, shape, dt=F32):
        return nc.dram_tensor(name, list(shape), dt, kind="ExternalInput")

    NTILE = S // 128
    x_keys = din("x_keys", [S, D])
    x_own = din("x_own", [NOWN, D])
    w_q = din("w_q", [D, 1024])
    w_k = din("w_k", [D, 1024])
    w_v = din("w_v", [D, 1024])
    kbias = din("kbias", [128, NTILE])
    w_conv = din("w_conv", [D, 2048])
    g1 = din("g1", [128, 16])
    cwk = din("cwk", [1024, 31])
    cbias = din("cbias", [128, 8])
    lng = din("lng", [128, 8])
    lnb = din("lnb", [128, 8])
    qkg = din("qkg", [1, 256])
    lam4 = din("lam4", [1, 256])
    sgn = din("sgn", [128, 1])
    w_out = din("w_out", [D, D])
    g2 = din("g2", [128, 16])
    w_rt = din("w_rt", [128, 16 * 36])
    b_rt = din("b_rt", [1, 36])
    w_gate = din("w_gate", [NEXP_RUN, D, DEXP])
    w_up = din("w_up", [NEXP_RUN, D, DEXP])
    w_down = din("w_down", [NEXP_RUN, DEXP, D])
    ident_in = din("ident", [128, 128])
    masks_in = din("masks", [128, 4 * 512])
    y_out = nc.dram_tensor("y", [NT, D], F32, kind="ExternalOutput")
    k_scr = nc.dram_tensor("k_scr", [8, 128, S], BF16)
    v_scr = nc.dram_tensor("v_scr", [8, 128, S], BF16)
    q_scr = nc.dram_tensor("q_scr", [8, 128, NT], BF16)
    ya_scr = nc.dram_tensor("ya_scr", [1024, NT], BF16)
    h_scr = nc.dram_tensor("h_scr", [NT, D], F32)

    ARENA_BYTES = 206 * 1024
    arena = stack.enter_context(nc.sbuf_tensor("arena", [128, ARENA_BYTES // 2], BF16))
    psb_t = [stack.enter_context(nc.psum_tensor(f"ps{i}", [128, 512], F32)) for i in range(8)]
    psb = [t[:, :] for t in psb_t]
    PS = [Buf(f"ps{i}") for i in range(8)]

    class Arena:
        def __init__(self):
            self.off = 0

        def alloc(self, nbytes, dt):
            a = self.off
            self.off += (nbytes + 63) // 64 * 64
            assert self.off <= ARENA_BYTES, f"arena overflow {self.off}"
            ap = arena[:, a // 2:(a + nbytes) // 2]
            if dt == F32:
                ap = ap.bitcast(F32)
            return ap

        def f32(self, n):
            return self.alloc(n * 4, F32)

        def bf(self, n):
            return self.alloc(n * 2, BF16)

    A = Arena()
    ident_f = A.f32(128)
    ident_b = A.bf(128)
    ones_f = A.f32(128)
    ones_b = A.bf(128)
    nhalf = A.f32(512)
    col = A.f32(64)
    B_const = Buf("const")
    CONST_END = None

    def dma(eng, out, in_, reads, writes, chan_buf):
        return P.add(eng, lambda h: h.dma_start(out=out, in_=in_), reads=reads, writes=writes,
                     kind="d", chan_buf=chan_buf)

    B_identf = Buf("identf")
    dma("sp", ident_f, ident_in.ap(), [], [B_identf], B_identf)
    P.add("dve", lambda h: h.tensor_copy(ident_b, ident_f), reads=[B_identf], writes=[B_const])
    P.add("dve", lambda h: h.memset(ones_f, 1.0), writes=[B_const])
    P.add("dve", lambda h: h.memset(ones_b, 1.0), writes=[B_const])
    P.add("dve", lambda h: h.memset(nhalf, -0.5), writes=[B_const])
    CONST_END = A.off

    final_stores = []
    try:
        def rstd_ops(v_ap, out_ap, n, bufs_in, buf_out):
            P.add("pool", lambda h: h.tensor_tensor(out_ap, v_ap, nhalf[:, 0:n], ALU.pow),
                  reads=bufs_in + [B_const], writes=[buf_out])

        qkgt = A.f32(256)
        l4 = A.f32(256)
        lamt = A.f32(8)
        sgcol = A.f32(1)
        masks = A.bf(4 * 512)
        kbt = A.f32(NTILE)
        g1col = A.f32(16)
        CONST_END = A.off

        Wq = A.bf(16 * 1024)
        Wk = A.bf(16 * 1024)
        Wv = A.bf(16 * 1024)
        xs = [A.f32(D) for _ in range(2)]
        junk = A.bf(D)
        xn = [A.bf(D) for _ in range(2)]
        xnT = [A.bf(D) for _ in range(2)]
        smalls = [A.f32(64) for _ in range(2)]
        sqk = A.f32(1024)
        tmpk = A.f32(1024)
        kb_ = A.bf(1024)
        qb_ = A.bf(1024)
        kst = A.bf(8 * 512)
        vst = A.bf(8 * 512)
        qst = A.bf(8 * 512)

        B_xs = [Buf(f"xs{i}") for i in range(2)]
        B_junk = Buf("junk")
        B_xn = [Buf(f"xn{i}") for i in range(2)]
        B_xnT = [Buf(f"xnT{i}") for i in range(2)]
        B_Wq, B_Wk, B_Wv = Buf("Wq"), Buf("Wk"), Buf("Wv")
        B_g1col = Buf("g1col")
        B_sm = [Buf(f"sm{i}") for i in range(2)]
        B_sqk = Buf("sqk")
        B_tmpk = Buf("tmpk")
        B_kb = Buf("kb")
        B_qb = Buf("qb")
        B_kst, B_vst, B_qst = Buf("kst"), Buf("vst"), Buf("qst")
        B_misc = Buf("miscAB")

        dma("sp", g1col, g1.ap(), [], [B_g1col], B_g1col)
        dma("sp", qkgt, qkg.ap()[0:1, :].broadcast_to([128, 256]), [], [B_misc], B_misc)
        B_l4 = Buf("l4")
        dma("sp", l4, lam4.ap()[0:1, :].broadcast_to([128, 256]), [], [B_l4], B_l4)
        B_sg = Buf("sg")
        dma("sp", sgcol, sgn.ap(), [], [B_sg], B_sg)
        B_kbt = Buf("kbt")
        dma("sp", kbt, kbias.ap(), [], [B_kbt], B_kbt)
        B_mk = Buf("masks")
        dma("sp", xs[1], masks_in.ap(), [], [B_xs[1]], B_xs[1])
        P.add("dve", lambda h, src=xs[1]: h.tensor_copy(masks, src), reads=[B_xs[1]], writes=[B_mk])
        P.add("dve", lambda h: h.tensor_scalar(qkgt[:, 0:128], qkgt[:, 0:128], 0.125, None, ALU.mult),
              reads=[B_misc], writes=[B_misc])
        B_lam = Buf("lam")
        P.add("dve", lambda h: h.tensor_tensor(l4[:, 0:64], l4[:, 0:64], l4[:, 64:128], ALU.mult),
              reads=[B_l4], writes=[B_l4])
        P.add("dve", lambda h: h.tensor_tensor(l4[:, 128:192], l4[:, 128:192], l4[:, 192:256], ALU.mult),
              reads=[B_l4], writes=[B_l4])
        P.add("dve", lambda h: h.tensor_reduce(lamt[:, 0:1], l4[:, 0:64], AX.X, ALU.add),
              reads=[B_l4], writes=[B_lam])
        P.add("dve", lambda h: h.tensor_reduce(lamt[:, 1:2], l4[:, 128:192], AX.X, ALU.add),
              reads=[B_l4], writes=[B_lam])
        P.add("act", lambda h: h.activation(lamt[:, 2:4], lamt[:, 0:2], AF.Exp), reads=[B_lam], writes=[B_lam])
        P.add("dve", lambda h: h.tensor_tensor(lamt[:, 4:5], lamt[:, 3:4], lamt[:, 2:3], ALU.subtract),
              reads=[B_lam], writes=[B_lam])
        P.add("dve", lambda h: h.tensor_scalar(lamt[:, 5:6], lamt[:, 4:5], -LAM_INIT, None, ALU.add),
              reads=[B_lam], writes=[B_lam])
        neg_lam = lamt[:, 5:6]
        P.add("dve", lambda h: h.tensor_scalar(sgcol, sgcol, 1.0 - LAM_INIT, None, ALU.mult),
              reads=[B_sg], writes=[B_sg])

        wcnt = [0]

        def load_w1024(src, Wt, B_Wt):
            srcv = src.ap().rearrange("(k p) n -> p k n", p=128)
            Wtv = Wt.rearrange("p (k n) -> p k n", k=16)
            for k2 in range(8):
                s = wcnt[0] % 2
                wcnt[0] += 1
                dma("sp", xs[s].rearrange("p (k n) -> p k n", k=2), srcv[:, 2 * k2:2 * k2 + 2, :], [], [B_xs[s]], B_xs[s])
                dstv = Wtv[:, 2 * k2:2 * k2 + 2, :]
                srcs = xs[s].rearrange("p (k n) -> p k n", k=2)
                if k2 % 2 == 0:
                    P.add("act", lambda h, dstv=dstv, srcs=srcs: h.copy(dstv, srcs), reads=[B_xs[s]], writes=[B_Wt])
                else:
                    P.add("dve", lambda h, dstv=dstv, srcs=srcs: h.tensor_copy(dstv, srcs), reads=[B_xs[s]], writes=[B_Wt])

        load_w1024(w_k, Wk, B_Wk)
        load_w1024(w_v, Wv, B_Wv)
        load_w1024(w_q, Wq, B_Wq)

        def norm_transpose(x_dram_rows, xs_s, Bxs, xn_s, Bxn, sm, Bsm, jk, Bjk, gcol, B_gcol,
                           ps_a, ps_b, out_views, out_bufs):
            dma("sp", xs_s, x_dram_rows, [], [Bxs], Bxs)
            P.add("act", lambda h: h.activation(jk, xs_s, AF.Square, accum_out=sm[:, 0:1]),
                  reads=[Bxs], writes=[Bjk, Bsm])
            P.add("dve", lambda h: h.tensor_scalar(sm[:, 1:2], sm[:, 0:1], 1.0 / D, EPS, ALU.mult, ALU.add),
                  reads=[Bsm], writes=[Bsm])
            rstd_ops(sm[:, 1:2], sm[:, 2:3], 1, [Bsm], Bsm)
            P.add("act", lambda h: h.activation(xn_s, xs_s, AF.Copy, scale=sm[:, 2:3]),
                  reads=[Bxs, Bsm], writes=[Bxn])
            tpa = psb[ps_a].bitcast(BF16)
            tpb = psb[ps_b].bitcast(BF16)
            for kc in range(16):
                tp = (tpa if kc < 8 else tpb)[:, (kc % 8) * 128:(kc % 8 + 1) * 128]
                P.add("pe", lambda h, tp=tp, kc=kc: h.transpose(tp, xn_s[:, kc * 128:(kc + 1) * 128], ident_b),
                      reads=[Bxn, B_const], writes=[PS[ps_a if kc < 8 else ps_b]])
            for kc in range(16):
                tp = (tpa if kc < 8 else tpb)[:, (kc % 8) * 128:(kc % 8 + 1) * 128]
                ov = out_views[kc]
                P.add("dve", lambda h, tp=tp, kc=kc, ov=ov: h.tensor_scalar(ov, tp, gcol[:, kc:kc + 1], None, ALU.mult),
                      reads=[PS[ps_a if kc < 8 else ps_b], B_gcol], writes=out_bufs)

        def a_norm(ti):
            s = ti % 2
            xv = xnT[s].rearrange("p (k t) -> p k t", k=16)
            norm_transpose(x_keys.ap()[ti * 128:(ti + 1) * 128, :], xs[s], B_xs[s], xn[s], B_xn[s],
                           smalls[s], B_sm[s], junk, B_junk, g1col, B_g1col,
                           0, 1, [xv[:, kc, :] for kc in range(16)], [B_xnT[s]])

        def proj(ti, Wt, B_Wt, b0):
            s = ti % 2
            xv = xnT[s].rearrange("p (k t) -> p k t", k=16)
            Wtv = Wt.rearrange("p (k n) -> p k n", k=16)
            for half in range(2):
                for kc in range(16):
                    P.add("pe", lambda h, kc=kc, xv=xv, half=half, Wtv=Wtv: h.matmul(
                        psb[b0 + half], xv[:, kc, :], Wtv[:, kc, half * 512:(half + 1) * 512],
                        start=(kc == 0), stop=(kc == 15)),
                        reads=[B_xnT[s], B_Wt], writes=[PS[b0 + half]])

        def qk_norm(ti, b0, goff, dst, B_dst):
            s = ti % 2
            sm = smalls[s]
            for half in range(2):
                P.add("act", lambda h, half=half: h.activation(sqk[:, half * 512:(half + 1) * 512], psb[b0 + half], AF.Square),
                      reads=[PS[b0 + half]], writes=[B_sqk])
                P.add("dve", lambda h, half=half, sm=sm: h.tensor_reduce(
                    sm[:, 4 + 8 * half:12 + 8 * half], sqk[:, half * 512:(half + 1) * 512].rearrange("p (g d) -> p g d", g=8),
                    AX.X, ALU.add), reads=[B_sqk], writes=[B_sm[s]])
            P.add("dve", lambda h, sm=sm: h.tensor_scalar(sm[:, 20:36], sm[:, 4:20], 1.0 / 64, EPS, ALU.mult, ALU.add),
                  reads=[B_sm[s]], writes=[B_sm[s]])
            rstd_ops(sm[:, 20:36], sm[:, 36:52], 16, [B_sm[s]], B_sm[s])
            for half in range(2):
                P.add("dve", lambda h, half=half, sm=sm: h.tensor_tensor(
                    tmpk[:, half * 512:(half + 1) * 512].rearrange("p (g d) -> p g d", g=8),
                    psb[b0 + half].rearrange("p (g d) -> p g d", g=8),
                    sm[:, 36 + 8 * half:44 + 8 * half].unsqueeze(2).broadcast_to([128, 8, 64]), ALU.mult),
                    reads=[PS[b0 + half], B_sm[s]], writes=[B_tmpk])
                P.add("dve", lambda h, half=half: h.tensor_tensor(
                    dst[:, half * 512:(half + 1) * 512].rearrange("p (g f) -> p g f", g=4),
                    tmpk[:, half * 512:(half + 1) * 512].rearrange("p (g f) -> p g f", g=4),
                    qkgt[:, goff:goff + 128].unsqueeze(1).broadcast_to([128, 4, 128]), ALU.mult),
                    reads=[B_tmpk, B_misc], writes=[B_dst])

        def head_transposes(src, B_src, bank, stage, B_stage, tl):
            tq = psb[bank].bitcast(BF16)
            for hh in range(8):
                P.add("pe", lambda h, hh=hh, tq=tq: h.transpose(tq[:, hh * 128:(hh + 1) * 128], src[:, hh * 128:(hh + 1) * 128], ident_b),
                      reads=[B_src, B_const], writes=[PS[bank]])
            sv = stage.rearrange("p (g t) -> p g t", g=8)[:, :, tl * 128:(tl + 1) * 128]
            P.add("act", lambda h, tq=tq, sv=sv: h.copy(sv, tq.rearrange("p (g t) -> p g t", g=8)),
                  reads=[PS[bank]], writes=[B_stage])

        if stop_after == "A0":
            raise _Stop()
        NQT = NT // 128
        a_norm(0)
        for ti in range(NTILE):
            tl = ti % 4
            proj(ti, Wk, B_Wk, 2)
            if ti + 1 < NTILE:
                a_norm(ti + 1)
            qk_norm(ti, 2, 128, kb_, B_kb)
            proj(ti, Wv, B_Wv, 4)
            vsv = vst.rearrange("p (g t d) -> p g t d", g=8, t=4)
            for half in range(2):
                P.add("act", lambda h, half=half, tl=tl, vsv=vsv: h.copy(
                    vsv[:, 4 * half:4 * half + 4, tl, :], psb[4 + half].rearrange("p (g d) -> p g d", g=4)),
                    reads=[PS[4 + half]], writes=[B_vst])
            if ti < NQT:
                proj(ti, Wq, B_Wq, 2)
                qk_norm(ti, 2, 0, qb_, B_qb)
            head_transposes(kb_, B_kb, 6, kst, B_kst, tl)
            if ti < NQT:
                head_transposes(qb_, B_qb, 7, qst, B_qst, tl)
            if tl == 3:
                t0 = (ti - 3) * 128
                dma("sp", k_scr.ap()[:, :, t0:t0 + 512].rearrange("g p t -> p g t"), kst.rearrange("p (g t) -> p g t", g=8),
                    [B_kst], [Buf(f"kscr{ti}")], B_kst)
                dma("sp", v_scr.ap()[:, :, t0:t0 + 512].rearrange("g p t -> p g t"), vst.rearrange("p (g t) -> p g t", g=8),
                    [B_vst], [Buf(f"vscr{ti}")], B_vst)
                if ti < NQT:
                    dma("sp", q_scr.ap()[:, :, t0:t0 + 512].rearrange("g p t -> p g t"), qst.rearrange("p (g t) -> p g t", g=8),
                        [B_qst], [Buf(f"qscr{ti}")], B_qst)
        P.barrier()

        if stop_after == "A":
            raise _Stop()
        A.off = CONST_END
        kTh = [A.bf(S) for _ in range(2)]
        Vh = [A.bf(S) for _ in range(2)]
        qTh = [A.bf(NT) for _ in range(2)]
        pT = [A.bf(512) for _ in range(4)]
        dacc = [A.f32(512) for _ in range(2)]
        rd = [A.f32(512) for _ in range(2)]
        osb = [A.f32(512) for _ in range(3)]
        sqo = A.f32(512)
        vrs = A.f32(512)
        rs = A.f32(512)
        yb = [A.bf(512) for _ in range(2)]
        B_kTh = [Buf("kTh0"), Buf("kTh1")]
        B_Vh = [Buf("Vh0"), Buf("Vh1")]
        B_qTh = [Buf("qTh0"), Buf("qTh1")]
        B_pT = [Buf(f"pT{i}") for i in range(4)]
        B_dacc = [Buf("dacc0"), Buf("dacc1")]
        B_rd = [Buf(f"rd{i}") for i in range(2)]
        B_os = [Buf(f"os{i}") for i in range(3)]
        B_sqo = Buf("sqo")
        B_vrs = Buf("vrs")
        B_rs = Buf("rs")
        B_yb = [Buf(f"yb{i}") for i in range(2)]

        def load_head(hd):
            s = hd % 2
            dma("sp", kTh[s], k_scr.ap()[hd], [], [B_kTh[s]], B_kTh[s])
            dma("sp", Vh[s], v_scr.ap()[hd], [], [B_Vh[s]], B_Vh[s])
            dma("sp", qTh[s], q_scr.ap()[hd], [], [B_qTh[s]], B_qTh[s])

        cnt = 0
        load_head(0)
        for hd in range(8):
            hs = hd % 2
            if hd + 1 < 8:
                load_head(hd + 1)
            kT, Vt, qT = kTh[hs], Vh[hs], qTh[hs]
            B_kT, B_V, B_qT = B_kTh[hs], B_Vh[hs], B_qTh[hs]
            for qt in range(NT // 512):
                ktiles = list(range(4 * (qt + 1))) + list(range(NQT, NTILE))
                nk = len(ktiles)
                LOOK = 2

                def emit_qk(ki, kt, r, sb, pb):
                    P.add("pe", lambda h, sb=sb, r=r, kt=kt, qt=qt, kT=kT, qT=qT: h.matmul(
                        psb[sb], kT[64 * r:64 * r + 64, kt * 128:(kt + 1) * 128],
                        qT[64 * r:64 * r + 64, qt * 512:(qt + 1) * 512], start=True, stop=True),
                        reads=[B_kT, B_qT], writes=[PS[sb]])
                    if kt < NQT:
                        P.add("act", lambda h, sb=sb, pb=pb: h.activation(pT[pb], psb[sb], AF.Exp),
                              reads=[PS[sb]], writes=[B_pT[pb]])
                        if kt >= 4 * qt:
                            j = kt - 4 * qt
                            P.add("pool", lambda h, pb=pb, j=j: h.tensor_tensor(pT[pb], pT[pb], masks[:, j * 512:(j + 1) * 512], ALU.mult),
                                  reads=[B_pT[pb], B_mk], writes=[B_pT[pb]])
                    else:
                        P.add("act", lambda h, sb=sb, pb=pb, kt=kt: h.activation(pT[pb], psb[sb], AF.Exp, bias=kbt[:, kt:kt + 1]),
                              reads=[PS[sb], B_kbt], writes=[B_pT[pb]])

                def emit_pv(ki, kt, r, sb, pb):
                    P.add("pe", lambda h, pb=pb, r=r, kt=kt, ki=ki, nk=nk, Vt=Vt: h.matmul(
                        psb[3 + r], Vt[:, kt * 128:(kt + 1) * 128], pT[pb], start=(ki == 0), stop=(ki == nk - 1)),
                        reads=[B_V, B_pT[pb]], writes=[PS[3 + r]])
                    deng = "dve" if r == 0 else "pool"
                    if ki == 0:
                        P.add(deng, lambda h, pb=pb, r=r: h.tensor_copy(dacc[r], pT[pb]),
                              reads=[B_pT[pb]], writes=[B_dacc[r]])
                    else:
                        P.add(deng, lambda h, pb=pb, r=r: h.tensor_tensor(dacc[r], dacc[r], pT[pb], ALU.add),
                              reads=[B_pT[pb], B_dacc[r]], writes=[B_dacc[r]])

                pend = []
                for ki, kt in enumerate(ktiles):
                    for r in range(2):
                        it = (ki, kt, r, cnt % 3, cnt % 4)
                        cnt += 1
                        emit_qk(*it)
                        pend.append(it)
                        if len(pend) > LOOK:
                            emit_pv(*pend.pop(0))
                while pend:
                    emit_pv(*pend.pop(0))
                for r in range(2):
                    P.add("pe", lambda h, r=r: h.matmul(psb[5 + r], ones_f, dacc[r], start=True, stop=True),
                          reads=[B_const, B_dacc[r]], writes=[PS[5 + r]])
                    P.add("dve", lambda h, r=r: h.reciprocal(rd[r], psb[5 + r]), reads=[PS[5 + r]], writes=[B_rd[r]])
                    P.add("dve", lambda h, r=r: h.tensor_tensor(osb[r], psb[3 + r], rd[r], ALU.mult),
                          reads=[PS[3 + r], B_rd[r]], writes=[B_os[r]])
                P.add("dve", lambda h: h.scalar_tensor_tensor(osb[2], osb[1], neg_lam, osb[0], ALU.mult, ALU.add),
                      reads=[B_os[0], B_os[1], B_lam], writes=[B_os[2]])
                P.add("act", lambda h: h.activation(sqo, osb[2], AF.Square), reads=[B_os[2]], writes=[B_sqo])
                P.add("pe", lambda h: h.matmul(psb[7], ones_f, sqo, start=True, stop=True),
                      reads=[B_const, B_sqo], writes=[PS[7]])
                P.add("dve", lambda h: h.tensor_scalar(vrs, psb[7], 1.0 / 128, EPS, ALU.mult, ALU.add),
                      reads=[PS[7]], writes=[B_vrs])
                rstd_ops(vrs, rs, 512, [B_vrs], B_rs)
                P.add("dve", lambda h: h.tensor_tensor(osb[2], osb[2], rs, ALU.mult),
                      reads=[B_os[2], B_rs], writes=[B_os[2]])
                ys = qt % 2
                P.add("act", lambda h, ys=ys: h.activation(yb[ys], osb[2], AF.Copy, scale=sgcol),
                      reads=[B_os[2], B_sg], writes=[B_yb[ys]])
                dma("sp", ya_scr.ap()[hd * 128:(hd + 1) * 128, qt * 512:(qt + 1) * 512], yb[ys], [B_yb[ys]],
                    [Buf(f"yascr{hd}_{qt}")], B_yb[ys])
        P.barrier()

        if stop_after == "B":
            raise _Stop()
        A.off = CONST_END
        W64 = A.bf(16 * 2048)
        xs2 = [A.f32(D) for _ in range(2)]
        junk2 = A.bf(D)
        xn2 = [A.bf(D) for _ in range(2)]
        smalls2 = [A.f32(16) for _ in range(2)]
        _r1 = A.off
        xnTg = A.bf(16 * 512)
        hTt = A.f32(8 * 544)
        _r1_end = A.off
        accs = A.f32(8 * 512)
        ycT = A.bf(8 * NT)
        assert _r1_end - _r1 >= 8 * NT * 2
        yaT = arena[:, _r1 // 2:_r1 // 2 + 8 * NT]
        sig = A.f32(512)
        sqc = A.f32(512)
        mean = A.f32(512)
        msq = A.f32(512)
        var = A.f32(512)
        rstdc = A.f32(512)
        zt = A.f32(512)
        cwT = A.f32(8 * 31)
        cbc = A.f32(8)
        lngc = A.f32(8)
        lnbc = A.f32(8)
        g1c2 = A.f32(16)
        D_END = A.off

        xs[0], xs[1] = xs2
        xn[0], xn[1] = xn2
        smalls[0], smalls[1] = smalls2
        junk = junk2
        B_xs = [Buf("xs2a"), Buf("xs2b")]
        B_xn = [Buf("xn2a"), Buf("xn2b")]
        B_sm = [Buf("sm2a"), Buf("sm2b")]
        B_junk = Buf("junk2")

        B_W64 = Buf("W64")
        B_g1c2 = Buf("g1c2")
        B_small = Buf("smallD")
        B_xnTg = Buf("xnTg")
        B_hT = Buf("hT")
        B_accs = [Buf(f"accs{i}") for i in range(8)]
        B_ycT = Buf("ycT")
        B_yaT = Buf("yaT")
        B_sig = Buf("sig")
        B_sqc = Buf("sqc")
        B_stat = Buf("stat")
        B_zt = Buf("zt")

        dma("sp", g1c2, g1.ap(), [], [B_g1c2], B_g1c2)
        dma("sp", cwT.rearrange("p (c j) -> p c j", c=8), cwk.ap().rearrange("(c p) j -> p c j", p=128), [], [B_small], B_small)
        B_s2 = Buf("s2")
        dma("sp", cbc, cbias.ap(), [], [B_s2], B_s2)
        B_s3 = Buf("s3")
        dma("sp", lngc, lng.ap(), [], [B_s3], B_s3)
        B_s4 = Buf("s4")
        dma("sp", lnbc, lnb.ap(), [], [B_s4], B_s4)

        W64v = W64.rearrange("p (k n) -> p k n", k=16)

        def load_w64(src):
            srcv = src.ap().rearrange("(k p) n -> p k n", p=128)
            for kc in range(16):
                s = kc % 2
                dma("sp", xs[s], srcv[:, kc, :], [], [B_xs[s]], B_xs[s])
                eng = "act" if kc % 2 == 0 else "dve"
                if eng == "act":
                    P.add("act", lambda h, s=s, kc=kc: h.copy(W64v[:, kc, :], xs[s]), reads=[B_xs[s]], writes=[B_W64])
                else:
                    P.add("dve", lambda h, s=s, kc=kc: h.tensor_copy(W64v[:, kc, :], xs[s]), reads=[B_xs[s]], writes=[B_W64])

        load_w64(w_conv)
        hTv = hTt.rearrange("p (c t) -> p c t", c=8)
        accv = accs.rearrange("p (c t) -> p c t", c=8)
        ycv = ycT.rearrange("p (c t) -> p c t", c=8)
        yav = yaT.rearrange("p (c t) -> p c t", c=8)
        xgv = xnTg.rearrange("p (k t) -> p k t", k=16)
        cwv = cwT.rearrange("p (c j) -> p c j", c=8)
        P.add("dve", lambda h: h.memset(hTt, 0.0), writes=[B_hT])

        groups = [(0, 128, True)] + [(HALO + 512 * j, 512, False) for j in range(NGRP)]
        for gi, (r0, ntok, is_halo) in enumerate(groups):
            for t in range(ntok // 128):
                norm_transpose(x_own.ap()[r0 + t * 128:r0 + (t + 1) * 128, :], xs[t % 2], B_xs[t % 2],
                               xn[t % 2], B_xn[t % 2], smalls[t % 2], B_sm[t % 2], junk, B_junk, g1c2, B_g1c2, 0, 1,
                               [xgv[:, kc, t * 128:(t + 1) * 128] for kc in range(16)], [B_xnTg])
            if gi >= 1:
                pn = groups[gi - 1][1]
                for c in range(8):
                    P.add("dve", lambda h, c=c, pn=pn: h.tensor_copy(hTv[:, c, 2:32], hTv[:, c, 32 + pn - 30:32 + pn]),
                          reads=[B_hT], writes=[B_hT])
            for c in range(8):
                for half in range(2):
                    pb_ = 2 + half
                    cofs = half * 1024 + c * 128
                    for kc in range(16):
                        P.add("pe", lambda h, pb_=pb_, kc=kc, cofs=cofs, ntok=ntok: h.matmul(
                            psb[pb_][:, 0:ntok], W64v[:, kc, cofs:cofs + 128], xgv[:, kc, 0:ntok],
                            start=(kc == 0), stop=(kc == 15)),
                            reads=[B_W64, B_xnTg], writes=[PS[pb_]])
                P.add("act", lambda h, ntok=ntok: h.activation(sig[:, 0:ntok], psb[3][:, 0:ntok], AF.Sigmoid),
                      reads=[PS[3]], writes=[B_sig])
                P.add("dve", lambda h, c=c, ntok=ntok: h.tensor_tensor(hTv[:, c, 32:32 + ntok], psb[2][:, 0:ntok], sig[:, 0:ntok], ALU.mult),
                      reads=[PS[2], B_sig], writes=[B_hT])
            if is_halo:
                continue
            j = gi - 1
            for c in range(8):
                eng = "dve"
                P.add(eng, lambda h, c=c: h.tensor_scalar(accv[:, c, :], hTv[:, c, 2:2 + 512], cwv[:, c, 0:1], cbc[:, c:c + 1], ALU.mult, ALU.add),
                      reads=[B_hT, B_small, B_s2], writes=[B_accs[c]])
                for k in range(1, 31):
                    P.add(eng, lambda h, c=c, k=k: h.scalar_tensor_tensor(accv[:, c, :], hTv[:, c, 2 + k:2 + k + 512], cwv[:, c, k:k + 1], accv[:, c, :], ALU.mult, ALU.add),
                          reads=[B_hT, B_small, B_accs[c]], writes=[B_accs[c]])
            for c in range(8):
                P.add("act", lambda h, c=c: h.activation(sqc, accv[:, c, :], AF.Square), reads=[B_accs[c]], writes=[B_sqc])
                P.add("pe", lambda h, c=c: h.matmul(psb[4], ones_f, accv[:, c, :], start=(c == 0), stop=(c == 7)),
                      reads=[B_const, B_accs[c]], writes=[PS[4]])
                P.add("pe", lambda h, c=c: h.matmul(psb[5], ones_f, sqc, start=(c == 0), stop=(c == 7)),
                      reads=[B_const, B_sqc], writes=[PS[5]])
            P.add("dve", lambda h: h.tensor_scalar(mean, psb[4], 1.0 / 1024, None, ALU.mult), reads=[PS[4]], writes=[B_stat])
            P.add("dve", lambda h: h.tensor_tensor(msq, mean, mean, ALU.mult), reads=[B_stat], writes=[B_stat])
            P.add("dve", lambda h: h.scalar_tensor_tensor(var, psb[5], 1.0 / 1024, msq, ALU.mult, ALU.subtract),
                  reads=[PS[5], B_stat], writes=[B_stat])
            P.add("dve", lambda h: h.tensor_scalar(var, var, EPS, None, ALU.add), reads=[B_stat], writes=[B_stat])
            rstd_ops(var, rstdc, 512, [B_stat], B_stat)
            for c in range(8):
                P.add("dve", lambda h, c=c: h.tensor_tensor(zt, accv[:, c, :], mean, ALU.subtract),
                      reads=[B_accs[c], B_stat], writes=[B_zt])
                P.add("dve", lambda h: h.tensor_tensor(zt, zt, rstdc, ALU.mult), reads=[B_zt, B_stat], writes=[B_zt])
                P.add("act", lambda h, c=c, j=j: h.activation(ycv[:, c, j * 512:(j + 1) * 512], zt, AF.Silu,
                                                            bias=lnbc[:, c:c + 1], scale=lngc[:, c:c + 1]),
                      reads=[B_zt, B_s3, B_s4], writes=[B_ycT])

        if stop_after == "D1":
            raise _Stop()
        P.barrier()
        for hh in range(8):
            dma("sp", yav[:, hh, :], ya_scr.ap()[hh * 128:(hh + 1) * 128, :], [], [B_yaT], B_yaT)

        load_w64(w_out)
        B_hs = [Buf("hs0"), Buf("hs1")]
        for tt in range(NT // 128):
            s = tt % 2
            dma("sp", xs[s], x_own.ap()[HALO + tt * 128:HALO + (tt + 1) * 128, :], [], [B_xs[s]], B_xs[s])
            for cg in range(4):
                pbk = 4 + cg
                for kc in range(16):
                    src = ycv[:, kc, tt * 128:(tt + 1) * 128] if kc < 8 else yav[:, kc - 8, tt * 128:(tt + 1) * 128]
                    P.add("pe", lambda h, pbk=pbk, kc=kc, src=src, cg=cg: h.matmul(
                        psb[pbk], src, W64v[:, kc, cg * 512:(cg + 1) * 512], start=(kc == 0), stop=(kc == 15)),
                        reads=[B_ycT, B_yaT, B_W64], writes=[PS[pbk]])
                P.add("dve", lambda h, pbk=pbk, s=s, cg=cg: h.tensor_tensor(xs[s][:, cg * 512:(cg + 1) * 512], xs[s][:, cg * 512:(cg + 1) * 512], psb[pbk], ALU.add),
                      reads=[PS[pbk], B_xs[s]], writes=[B_xs[s]])
            B_hrow = Buf(f"hrow{tt}")
            dma("sp", h_scr.ap()[tt * 128:(tt + 1) * 128, :], xs[s], [B_xs[s]], [B_hrow], B_xs[s])
        P.barrier()

        if stop_after == "D":
            raise _Stop()
        A.off = CONST_END
        NPASS = NT // 512
        TPP = 4
        acc = [A.f32(D) for _ in range(TPP)]
        tT = A.bf(16 * 512)
        Wsl = [A.bf(16 * 512) for _ in range(4)]
        st = [A.f32(D) for _ in range(4)]
        hTm = A.bf(4 * 512)
        junk3 = A.bf(D)
        tn32 = A.f32(D)
        hib = A.bf(D)
        lob = A.bf(D)
        thT = A.bf(D)
        tlT = A.bf(D)
        Wg32 = A.f32(16 * 36)
        Whb = A.bf(16 * 36)
        Wlb = A.bf(16 * 36)
        sgl = [A.f32(512) for _ in range(2)]
        cwt = A.f32(TPP * 32)
        Wr32 = A.f32(16 * 36)
        brt = A.f32(36)
        g2col = A.f32(16)
        lg = A.f32(36)
        rt = A.f32(64)
        sm3 = A.f32(8)

        B_acc = [Buf(f"acc{i}") for i in range(TPP)]
        B_tT = Buf("tT")
        B_W = [Buf(f"Wsl{i}") for i in range(4)]
        B_st = [Buf(f"st{i}") for i in range(4)]
        B_hTm = Buf("hTm")
        B_junk3 = Buf("junk3")
        B_tn32 = Buf("tn32")
        B_hib, B_lob, B_thT, B_tlT = Buf("hib"), Buf("lob"), Buf("thT"), Buf("tlT")
        B_sgl = [Buf("sgl0"), Buf("sgl1")]
        B_cwt = Buf("cwt")
        B_Wr = Buf("Wr32")
        B_brt = Buf("brt")
        B_g2 = Buf("g2col")
        B_lg = Buf("lg")
        B_rt = Buf("rt")
        B_sm3 = Buf("sm3")

        dma("sp", Wr32, w_rt.ap(), [], [B_Wr], B_Wr)
        dma("sp", brt, b_rt.ap()[0:1, :].broadcast_to([128, 36]), [], [B_brt], B_brt)
        dma("sp", g2col, g2.ap(), [], [B_g2], B_g2)
        if stop_after == "E0a":
            raise _Stop()
        Wrv = Wr32.rearrange("p (k n) -> p k n", k=16)
        tTv = tT.rearrange("p (k t) -> p k t", k=16)
        P.add("dve", lambda h: h.tensor_tensor(Wg32.rearrange("p (k n) -> p k n", k=16), Wrv,
                                               g2col.unsqueeze(2).broadcast_to([128, 16, 36]), ALU.mult),
              reads=[B_Wr, B_g2], writes=[B_Wr])
        P.add("act", lambda h: h.copy(Whb, Wg32), reads=[B_Wr], writes=[B_Wr])
        P.add("dve", lambda h: h.tensor_tensor(Wlb, Wg32, Whb, ALU.subtract), reads=[B_Wr], writes=[B_Wr])
        Whv = Whb.rearrange("p (k n) -> p k n", k=16)
        Wlv = Wlb.rearrange("p (k n) -> p k n", k=16)
        hmv = hTm.rearrange("p (f t) -> p f t", f=4)
        cwtv = cwt.rearrange("p (t e) -> p t e", t=TPP)
        wq = [0]
        sq_ = [0]
        final_stores = []

        def load_expert_matrix(src_view, npieces, piece_view_fn):
            slot = wq[0] % 4
            wq[0] += 1
            for pc in range(npieces):
                s = sq_[0] % 4
                sq_[0] += 1
                dma("sp", piece_view_fn(st[s]), src_view(pc), [], [B_st[s]], B_st[s])
                dst = Wsl[slot][:, pc * 2048:(pc + 1) * 2048]
                if pc % 2 == 0:
                    P.add("act", lambda h, s=s, dst=dst: h.copy(dst, st[s]), reads=[B_st[s]], writes=[B_W[slot]])
                else:
                    P.add("pool", lambda h, s=s, dst=dst: h.tensor_copy(dst, st[s]), reads=[B_st[s]], writes=[B_W[slot]])
            return slot

        for ps_ in range(NPASS):
            for tt in range(TPP):
                row0 = (ps_ * TPP + tt) * 128
                dma("sp", acc[tt], h_scr.ap()[row0:row0 + 128, :], [], [B_acc[tt]], B_acc[tt])
                P.add("act", lambda h, tt=tt: h.activation(junk3, acc[tt], AF.Square, accum_out=sm3[:, 0:1]),
                      reads=[B_acc[tt]], writes=[B_junk3, B_sm3])
                P.add("dve", lambda h: h.tensor_scalar(sm3[:, 1:2], sm3[:, 0:1], 1.0 / D, EPS, ALU.mult, ALU.add),
                      reads=[B_sm3], writes=[B_sm3])
                rstd_ops(sm3[:, 1:2], sm3[:, 2:3], 1, [B_sm3], B_sm3)
                P.add("act", lambda h, tt=tt: h.activation(tn32, acc[tt], AF.Copy, scale=sm3[:, 2:3]),
                      reads=[B_acc[tt], B_sm3], writes=[B_tn32])
                if stop_after == "E0b":
                    raise _Stop()
                P.add("act", lambda h: h.copy(hib, tn32), reads=[B_tn32], writes=[B_hib])
                P.add("dve", lambda h: h.tensor_tensor(lob, tn32, hib, ALU.subtract), reads=[B_tn32, B_hib], writes=[B_lob])
                for (srcb, B_srcb, bk) in ((hib, B_hib, 0), (lob, B_lob, 2)):
                    for kc in range(16):
                        tpv = psb[bk + kc // 8].bitcast(BF16)[:, (kc % 8) * 128:(kc % 8 + 1) * 128]
                        P.add("pe", lambda h, kc=kc, tpv=tpv, srcb=srcb: h.transpose(tpv, srcb[:, kc * 128:(kc + 1) * 128], ident_b),
                              reads=[B_srcb, B_const], writes=[PS[bk + kc // 8]])
                for hf in range(2):
                    P.add("dve", lambda h, hf=hf: h.tensor_copy(thT[:, hf * 1024:(hf + 1) * 1024], psb[0 + hf].bitcast(BF16)),
                          reads=[PS[0 + hf]], writes=[B_thT])
                    P.add("act", lambda h, hf=hf: h.copy(tlT[:, hf * 1024:(hf + 1) * 1024], psb[2 + hf].bitcast(BF16)),
                          reads=[PS[2 + hf]], writes=[B_tlT])
                P.add("pool", lambda h, tt=tt: h.tensor_tensor(tTv[:, :, tt * 128:(tt + 1) * 128], thT.rearrange("p (k t) -> p k t", k=16),
                                                             g2col.unsqueeze(2).broadcast_to([128, 16, 128]), ALU.mult),
                      reads=[B_thT, B_g2], writes=[B_tT])
                if stop_after == "E1a":
                    raise _Stop()
                thv = thT.rearrange("p (k t) -> p k t", k=16)
                tlv = tlT.rearrange("p (k t) -> p k t", k=16)
                combos = [(thv, B_thT, Whv), (thv, B_thT, Wlv), (tlv, B_tlT, Whv), (tlv, B_tlT, Wlv)]
                for ci, (tv_, B_tv, wv_) in enumerate(combos):
                    for kc in range(16):
                        P.add("pe", lambda h, kc=kc, tv_=tv_, wv_=wv_, ci=ci: h.matmul(
                            psb[4][:, 0:36], tv_[:, kc, :], wv_[:, kc, :], start=(ci == 0 and kc == 0), stop=(ci == 3 and kc == 15)),
                            reads=[B_tv, B_Wr], writes=[PS[4]])
                P.add("dve", lambda h: h.tensor_tensor(lg, psb[4][:, 0:36], brt, ALU.add), reads=[PS[4], B_brt], writes=[B_lg])
                if stop_after == "E1b":
                    raise _Stop()
                R = rt
                def dv(fn, reads, writes):
                    P.add("dve", fn, reads=reads, writes=writes)
                dv(lambda h: h.tensor_reduce(R[:, 0:1], lg[:, 0:4], AX.X, ALU.max), [B_lg], [B_rt])
                dv(lambda h: h.tensor_scalar(R[:, 1:2], R[:, 0:1], -1.0, None, ALU.mult), [B_rt], [B_rt])
                P.add("act", lambda h: h.activation(R[:, 56:60], lg[:, 0:4], AF.Exp, bias=R[:, 1:2], accum_out=R[:, 2:3]),
                      reads=[B_lg, B_rt], writes=[B_rt])
                dv(lambda h: h.reciprocal(R[:, 3:4], R[:, 2:3]), [B_rt], [B_rt])
                dv(lambda h: h.tensor_scalar(R[:, 4:8], lg[:, 0:4], R[:, 0:1], None, ALU.is_equal), [B_lg, B_rt], [B_rt])
                dv(lambda h: h.tensor_scalar(R[:, 8:16], lg[:, 4:12], R[:, 4:5], None, ALU.mult), [B_lg, B_rt], [B_rt])
                for g in range(1, 4):
                    dv(lambda h, g=g: h.scalar_tensor_tensor(R[:, 8:16], lg[:, 4 + 8 * g:12 + 8 * g], R[:, 4 + g:5 + g], R[:, 8:16], ALU.mult, ALU.add),
                       [B_lg, B_rt], [B_rt])
                dv(lambda h: h.tensor_reduce(R[:, 16:17], R[:, 8:16], AX.X, ALU.max), [B_rt], [B_rt])
                dv(lambda h: h.tensor_scalar(R[:, 17:25], R[:, 8:16], R[:, 16:17], None, ALU.is_equal), [B_rt], [B_rt])
                dv(lambda h: h.scalar_tensor_tensor(R[:, 25:33], R[:, 17:25], -1.0e30, R[:, 8:16], ALU.mult, ALU.add), [B_rt], [B_rt])
                dv(lambda h: h.tensor_reduce(R[:, 33:34], R[:, 25:33], AX.X, ALU.max), [B_rt], [B_rt])
                dv(lambda h: h.tensor_scalar(R[:, 34:42], R[:, 25:33], R[:, 33:34], None, ALU.is_equal), [B_rt], [B_rt])
                dv(lambda h: h.tensor_tensor(R[:, 42:43], R[:, 33:34], R[:, 16:17], ALU.subtract), [B_rt], [B_rt])
                P.add("act", lambda h: h.activation(R[:, 43:44], R[:, 42:43], AF.Exp), reads=[B_rt], writes=[B_rt])
                dv(lambda h: h.tensor_scalar(R[:, 44:45], R[:, 43:44], 1.0, None, ALU.add), [B_rt], [B_rt])
                dv(lambda h: h.reciprocal(R[:, 45:46], R[:, 44:45]), [B_rt], [B_rt])
                dv(lambda h: h.tensor_tensor(R[:, 46:47], R[:, 43:44], R[:, 45:46], ALU.mult), [B_rt], [B_rt])
                dv(lambda h: h.tensor_scalar(R[:, 48:56], R[:, 17:25], R[:, 45:46], None, ALU.mult), [B_rt], [B_rt])
                dv(lambda h: h.scalar_tensor_tensor(R[:, 48:56], R[:, 34:42], R[:, 46:47], R[:, 48:56], ALU.mult, ALU.add), [B_rt], [B_rt])
                dv(lambda h: h.tensor_scalar(R[:, 48:56], R[:, 48:56], R[:, 3:4], None, ALU.mult), [B_rt], [B_rt])
                for g in range(4):
                    dv(lambda h, g=g, tt=tt: h.tensor_scalar(cwtv[:, tt, 8 * g:8 * g + 8], R[:, 48:56], R[:, 4 + g:5 + g], None, ALU.mult),
                       [B_rt], [B_cwt])
            if stop_after == "E1":
                raise _Stop()
            for e in range(NEXP_RUN):
                gsl = load_expert_matrix(lambda pc, e=e: w_gate.ap()[e].rearrange("(k p) f -> p k f", p=128)[:, 4 * pc:4 * pc + 4, :],
                                         4, lambda stt: stt.rearrange("p (k f) -> p k f", k=4))
                usl = load_expert_matrix(lambda pc, e=e: w_up.ap()[e].rearrange("(k p) f -> p k f", p=128)[:, 4 * pc:4 * pc + 4, :],
                                         4, lambda stt: stt.rearrange("p (k f) -> p k f", k=4))
                dsl = load_expert_matrix(lambda pc, e=e: w_down.ap()[e][pc * 128:(pc + 1) * 128, :],
                                         4, lambda stt: stt)
                Wg = Wsl[gsl].rearrange("p (k f) -> p k f", k=16)
                Wu = Wsl[usl].rearrange("p (k f) -> p k f", k=16)
                Wd = Wsl[dsl].rearrange("p (f n) -> p f n", f=4)
                for fc in range(4):
                    gb = 0 + (fc % 2)
                    ub = 2 + (fc % 2)
                    for kc in range(16):
                        P.add("pe", lambda h, gb=gb, kc=kc, fc=fc, Wg=Wg: h.matmul(psb[gb], Wg[:, kc, fc * 128:(fc + 1) * 128], tTv[:, kc, :], start=(kc == 0), stop=(kc == 15)),
                              reads=[B_W[gsl], B_tT], writes=[PS[gb]])
                    for kc in range(16):
                        P.add("pe", lambda h, ub=ub, kc=kc, fc=fc, Wu=Wu: h.matmul(psb[ub], Wu[:, kc, fc * 128:(fc + 1) * 128], tTv[:, kc, :], start=(kc == 0), stop=(kc == 15)),
                              reads=[B_W[usl], B_tT], writes=[PS[ub]])
                    sl = fc % 2
                    P.add("act", lambda h, gb=gb, sl=sl: h.activation(sgl[sl], psb[gb], AF.Silu), reads=[PS[gb]], writes=[B_sgl[sl]])
                    P.add("dve", lambda h, ub=ub, sl=sl, fc=fc: h.tensor_tensor(hmv[:, fc, :], sgl[sl], psb[ub], ALU.mult),
                          reads=[B_sgl[sl], PS[ub]], writes=[B_hTm])
                oc = 0
                for tt in range(TPP):
                    for cg in range(4):
                        ob = 4 + (oc % 4)
                        oc += 1
                        for fc in range(4):
                            P.add("pe", lambda h, ob=ob, fc=fc, tt=tt, cg=cg, Wd=Wd: h.matmul(psb[ob], hmv[:, fc, tt * 128:(tt + 1) * 128], Wd[:, fc, cg * 512:(cg + 1) * 512], start=(fc == 0), stop=(fc == 3)),
                                  reads=[B_hTm, B_W[dsl]], writes=[PS[ob]])
                        P.add("dve", lambda h, ob=ob, tt=tt, cg=cg, e=e: h.scalar_tensor_tensor(
                            acc[tt][:, cg * 512:(cg + 1) * 512], psb[ob], cwtv[:, tt, e:e + 1], acc[tt][:, cg * 512:(cg + 1) * 512], ALU.mult, ALU.add),
                            reads=[PS[ob], B_cwt, B_acc[tt]], writes=[B_acc[tt]])
            for tt in range(TPP):
                row0 = (ps_ * TPP + tt) * 128
                B_yrow = Buf(f"yrow{row0}")
                final_stores.append(dma("sp", y_out.ap()[row0:row0 + 128, :], acc[tt], [B_acc[tt]], [B_yrow], B_acc[tt]))

    except _Stop:
        pass

    fin = P.add("sp", lambda h: h.nop(), reads=[], writes=[])
    for so in final_stores:
        fin.deps.append(so)

    P.finalize()
    with nc.Block() as block:
        @block.tensor
        def _(e):
            P.emit("pe", e)

        @block.scalar
        def _(e):
            P.emit("act", e)

        @block.vector
        def _(e):
            P.emit("dve", e)

        @block.gpsimd
        def _(e):
            P.emit("pool", e)

        @block.sync
        def _(e):
            P.emit("sp", e)
    stack.close()
    return nc


def _masks():
    k = np.arange(128)[:, None]
    q = np.arange(512)[None, :]
    m = np.zeros((128, 4 * 512), np.float32)
    for j in range(4):
        m[:, j * 512:(j + 1) * 512] = ((2 * j + k // 64) <= (q // 64)).astype(np.float32)
    return m


def make_in_maps(x, norm1_g, w_in, conv_dw_kernel, conv_dw_bias, conv_ln_g, conv_ln_b,
                 q_norm_g, k_norm_g, lambda_q1, lambda_k1, lambda_q2, lambda_k2, subln_g,
                 w_out, norm2_g, w_group, b_group, w_router, b_router, w_gate, w_up, w_down, nexp_run=NEXP):
    f = lambda a: np.ascontiguousarray(np.asarray(a, dtype=np.float32))
    S = int(np.asarray(x).shape[1])
    NT = S // NCORE
    NOWN = NT + HALO
    NTILE = S // 128
    x2 = f(x).reshape(S, D)
    w_in0 = f(w_in)[0]
    xpad = np.concatenate([np.zeros((HALO, D), np.float32), x2], axis=0)
    w_rt = np.concatenate([f(w_group)[0]] + [f(w_router)[0, g] for g in range(4)], axis=1)
    b_rt = np.concatenate([f(b_group)[0]] + [f(b_router)[0, g] for g in range(4)], axis=0)[None, :]
    common = dict(
        w_conv=f(w_in0[:, 0:2048]), w_q=f(w_in0[:, 2048:3072]), w_k=f(w_in0[:, 3072:4096]),
        w_v=f(w_in0[:, 4096:5120]), g1=f(f(norm1_g).reshape(16, 128).T),
        cwk=f(f(conv_dw_kernel)[0].T), cbias=f(f(conv_dw_bias).reshape(8, 128).T),
        lng=f(f(conv_ln_g).reshape(8, 128).T), lnb=f(f(conv_ln_b).reshape(8, 128).T),
        qkg=np.concatenate([f(q_norm_g).reshape(1, 128), f(k_norm_g).reshape(1, 128)], axis=1),
        lam4=np.concatenate([f(lambda_q1).reshape(1, 64), f(lambda_k1).reshape(1, 64),
                             f(lambda_q2).reshape(1, 64), f(lambda_k2).reshape(1, 64)], axis=1),
        sgn=f(f(subln_g).reshape(1, 128).T), w_out=f(w_out)[0], g2=f(f(norm2_g).reshape(16, 128).T),
        w_rt=f(w_rt.reshape(16, 128, 36).transpose(1, 0, 2).reshape(128, 16 * 36)), b_rt=f(b_rt), w_gate=f(f(w_gate)[0][:nexp_run]), w_up=f(f(w_up)[0][:nexp_run]),
        w_down=f(f(w_down)[0][:nexp_run]),
        ident=np.eye(128, dtype=np.float32), masks=_masks(),
    )
    maps = []
    for c in range(NCORE):
        m = dict(common)
        m["x_own"] = f(xpad[c * NT:c * NT + NOWN])
        order = [c] + [b for b in range(NCORE) if b != c]
        m["x_keys"] = f(np.concatenate([x2[b * NT:(b + 1) * NT] for b in order], axis=0))
        kb = np.zeros((128, NTILE), np.float32)
        for pos, b in enumerate(order):
            if b > c:
                kb[:, pos * (NT // 128):(pos + 1) * (NT // 128)] = -30000.0
        m["kbias"] = kb
        maps.append(m)
    return maps


_NC_CACHE = {}


def kernel(**inputs):
    if "nc" not in _NC_CACHE:
        _NC_CACHE["nc"] = build_program()
    nc = _NC_CACHE["nc"]
    maps = make_in_maps(**inputs)
    res = run_bass_kernel_spmd(nc, maps, core_ids=list(range(NCORE)))
    out = np.concatenate([np.asarray(r["y"], dtype=np.float32) for r in res.results], axis=0)
    return out.reshape(1, S_FULL, D)
```
